# Optimizing a Trainium2 kernel written in Bass

```python
import jax, jax.numpy as jnp
from jax import lax
import numpy as np

D_MODEL = 2048
BATCH = 8
SEQ = 4096
DEPTH = 4

N_MIXERS = 3
N_MLSTM_LAYERS = (DEPTH + 2) // 3
N_ATTN_LAYERS = (DEPTH + 1) // 3
N_RWKV_LAYERS = DEPTH // 3

D_FF = 5632
NORM_EPS = 1e-6

MLSTM_HEADS = 8
MLSTM_DQK = D_MODEL // (2 * MLSTM_HEADS)
MLSTM_DV = D_MODEL // MLSTM_HEADS
MLSTM_CHUNK = 64
MLSTM_SPLITS = (MLSTM_HEADS * MLSTM_DQK, 2 * MLSTM_HEADS * MLSTM_DQK,
                2 * MLSTM_HEADS * MLSTM_DQK + MLSTM_HEADS * MLSTM_DV,
                2 * MLSTM_HEADS * MLSTM_DQK + 2 * MLSTM_HEADS * MLSTM_DV,
                2 * MLSTM_HEADS * MLSTM_DQK + 2 * MLSTM_HEADS * MLSTM_DV + MLSTM_HEADS)
MLSTM_IN_COLS = 2 * MLSTM_HEADS * MLSTM_DQK + 2 * MLSTM_HEADS * MLSTM_DV + 2 * MLSTM_HEADS

ATTN_HEAD_DIM = 64
ATTN_Q_HEADS = D_MODEL // ATTN_HEAD_DIM
ATTN_GROUP = 8
ATTN_KV_HEADS = ATTN_Q_HEADS // ATTN_GROUP
ATTN_WINDOW = 128
ATTN_BLOCK = 128
ROPE_DIM = ATTN_HEAD_DIM // 4
ROPE_THETA = 500000.0
ATTN_QKV_COLS = (ATTN_Q_HEADS + 2 * ATTN_KV_HEADS) * ATTN_HEAD_DIM

RWKV_HEAD = 64
RWKV_HEADS = D_MODEL // RWKV_HEAD
RWKV_DECAY_LORA = 96
RWKV_AAA_LORA = 96
RWKV_GATE_LORA = 256
RWKV_LN_EPS = 64e-5

kernel_name = "hybrid_mlstm_swa_rwkv7_macaron"


def rmsnorm(x, gain):
    xf = x.astype(jnp.float32)
    y = xf * lax.rsqrt(jnp.mean(xf * xf, axis=-1, keepdims=True) + NORM_EPS)
    return (y * gain.astype(jnp.float32)).astype(x.dtype)


def swiglu(h, w_gu, w_down):
    gate, up = jnp.split(h @ w_gu, 2, axis=-1)
    return (jax.nn.silu(gate) * up) @ w_down


def rope_partial(x, cos, sin):
    half = ROPE_DIM // 2
    xf = x[..., :ROPE_DIM].astype(jnp.float32)
    x1, x2 = xf[..., :half], xf[..., half:]
    rot = jnp.concatenate([x1 * cos - x2 * sin, x2 * cos + x1 * sin], axis=-1).astype(x.dtype)
    return jnp.concatenate([rot, x[..., ROPE_DIM:]], axis=-1)


def mlstm_chunk_step(carry, xs):
    c_state, n_state, m_state = carry
    q, k, v, log_i, log_f = xs
    L = q.shape[2]
    causal = jnp.tril(jnp.ones((L, L), dtype=bool))
    b = jnp.cumsum(log_f, axis=-1)
    g = b[..., -1]
    log_d = jnp.where(causal, b[..., :, None] - b[..., None, :] + log_i[..., None, :], -jnp.inf)
    log_inter = b + m_state[..., None]
    m_out = jnp.maximum(log_inter, jnp.max(log_d, axis=-1))
    d_mat = jnp.exp(log_d - m_out[..., None])
    w_inter = jnp.exp(log_inter - m_out)
    s = jnp.einsum('bhtd,bhsd->bhts', q, k) * d_mat
    num = jnp.einsum('bhts,bhsv->bhtv', s, v) + w_inter[..., None] * jnp.einsum('bhvd,bhtd->bhtv', c_state, q)
    den = jnp.sum(s, axis=-1) + w_inter * jnp.einsum('bhd,bhtd->bht', n_state, q)
    h = num / jnp.maximum(jnp.abs(den), jnp.exp(-m_out))[..., None]
    log_w = g[..., None] - b + log_i
    m_new = jnp.maximum(g + m_state, jnp.max(log_w, axis=-1))
    w = jnp.exp(log_w - m_new[..., None])
    decay = jnp.exp(g + m_state - m_new)
    c_new = decay[..., None, None] * c_state + jnp.einsum('bhsv,bhsd->bhvd', v * w[..., None], k)
    n_new = decay[..., None] * n_state + jnp.einsum('bhs,bhsd->bhd', w, k)
    return (c_new, n_new, m_new), h


def mlstm_mixer(h, w_in, b_gate, head_gain, w_out):
    B, S, _ = h.shape
    H, DK, DV, L = MLSTM_HEADS, MLSTM_DQK, MLSTM_DV, MLSTM_CHUNK
    nc = S // L
    q, k, v, o, ig, fg = jnp.split(h @ w_in, MLSTM_SPLITS, axis=-1)

    def to_chunks(t, dh):
        return t.astype(jnp.float32).reshape(B, nc, L, H, dh).transpose(1, 0, 3, 2, 4)

    qc = to_chunks(q, DK)
    kc = to_chunks(k, DK) * (DK ** -0.5)
    vc = to_chunks(v, DV)
    bg = b_gate.astype(jnp.float32)
    log_i = (ig.astype(jnp.float32) + bg[0]).reshape(B, nc, L, H).transpose(1, 0, 3, 2)
    log_f = jax.nn.log_sigmoid(fg.astype(jnp.float32) + bg[1]).reshape(B, nc, L, H).transpose(1, 0, 3, 2)
    init = (jnp.zeros((B, H, DV, DK), jnp.float32), jnp.zeros((B, H, DK), jnp.float32),
            jnp.zeros((B, H), jnp.float32))
    _, hc = lax.scan(mlstm_chunk_step, init, (qc, kc, vc, log_i, log_f))
    hs = hc.transpose(1, 0, 3, 2, 4).reshape(B, S, H, DV)
    hs = rmsnorm(hs, head_gain).astype(h.dtype)
    return (hs.reshape(B, S, H * DV) * jax.nn.sigmoid(o)) @ w_out


def swa_sink_attention(h, positions, w_qkv, q_gain, k_gain, sinks, w_o):
    B, S, _ = h.shape
    HQ, HKV, G, DH, BLK = ATTN_Q_HEADS, ATTN_KV_HEADS, ATTN_GROUP, ATTN_HEAD_DIM, ATTN_BLOCK
    nb = S // BLK
    q, k, v = jnp.split(h @ w_qkv, (HQ * DH, (HQ + HKV) * DH), axis=-1)
    q = q.reshape(B, S, HQ, DH)
    k = k.reshape(B, S, HKV, DH)
    v = v.reshape(B, S, HKV, DH)
    inv_freq = ROPE_THETA ** (-jnp.arange(0, ROPE_DIM, 2, dtype=jnp.float32) / ROPE_DIM)
    ang = positions.astype(jnp.float32)[..., None] * inv_freq
    cos, sin = jnp.cos(ang)[:, :, None, :], jnp.sin(ang)[:, :, None, :]
    q = rope_partial(rmsnorm(q, q_gain), cos, sin)
    k = rope_partial(rmsnorm(k, k_gain), cos, sin)
    qb = q.reshape(B, nb, BLK, HKV, G, DH).transpose(1, 0, 2, 3, 4, 5)

    def banded(t):
        tp = jnp.pad(t, ((0, 0), (BLK, 0), (0, 0), (0, 0))).reshape(B, nb + 1, BLK, HKV, DH)
        return jnp.concatenate([tp[:, :-1], tp[:, 1:]], axis=2).transpose(1, 0, 2, 3, 4)

    kb, vb = banded(k), banded(v)
    qi = jnp.arange(BLK)[:, None] + BLK
    kj = jnp.arange(2 * BLK)[None, :]
    local_ok = (qi >= kj) & (qi - kj < ATTN_WINDOW)
    key_ok = local_ok[None] & ((jnp.arange(nb) > 0)[:, None, None] | (kj >= BLK)[None])
    sink_logit = sinks.astype(jnp.float32).reshape(HKV, G)[None, :, :, None, None]
    scale = DH ** -0.5

    def block(args):
        q_blk, k_blk, v_blk, ok = args
        s = jnp.einsum('bqhgd,bkhd->bhgqk', q_blk, k_blk).astype(jnp.float32) * scale
        s = jnp.where(ok, s, -jnp.inf)
        sink_col = jnp.broadcast_to(sink_logit, s.shape[:-1] + (1,))
        p = jax.nn.softmax(jnp.concatenate([s, sink_col], axis=-1), axis=-1)[..., :-1]
        return jnp.einsum('bhgqk,bkhd->bqhgd', p.astype(v_blk.dtype), v_blk)

    ob = lax.map(block, (qb, kb, vb, key_ok))
    o = ob.transpose(1, 0, 2, 3, 4, 5).reshape(B, S, HQ * DH)
    return o @ w_o


def rwkv7_step(state, xs):
    r, w, k, v, a_vec, b_vec = xs
    sa = jnp.einsum('bhvk,bhk->bhv', state, a_vec)
    state = state * w[:, :, None, :] + sa[..., None] * b_vec[:, :, None, :] + v[..., None] * k[:, :, None, :]
    y = jnp.einsum('bhvk,bhk->bhv', state, r)
    return state, y


def rwkv7_mixer(h, mix, w_rkv, w0, w_la, w_lb, a0, a_la, a_lb, g_la, g_lb, k_k, k_a, r_k, ln_w, ln_b, w_o):
    B, S, D = h.shape
    H, N = RWKV_HEADS, RWKV_HEAD
    dx = jnp.pad(h, ((0, 0), (1, 0), (0, 0)))[:, :-1] - h
    xr, xw, xk, xv, xa, xg = [h + dx * mix[i] for i in range(6)]
    r = xr @ w_rkv[0]
    k = xk @ w_rkv[1]
    v = xv @ w_rkv[2]
    log_w = -jax.nn.softplus(-(w0 + jnp.tanh(xw @ w_la) @ w_lb)) - 0.5
    a = jax.nn.sigmoid(a0 + (xa @ a_la) @ a_lb)
    g = jax.nn.sigmoid(xg @ g_la) @ g_lb

    def heads(t):
        return t.astype(jnp.float32).reshape(B, S, H, N)

    kk = heads(k * k_k)
    kk = kk / jnp.maximum(jnp.sqrt(jnp.sum(kk * kk, axis=-1, keepdims=True)), 1e-12)
    k = k * (1.0 + (a - 1.0) * k_a)
    r_h, k_h, v_h, a_h = heads(r), heads(k), heads(v), heads(a)
    decay = jnp.exp(-jnp.exp(heads(log_w)))

    def to_time(t):
        return t.transpose(1, 0, 2, 3)

    xs = tuple(map(to_time, (r_h, decay, k_h, v_h, -kk, kk * a_h)))
    _, y = lax.scan(rwkv7_step, jnp.zeros((B, H, N, N), jnp.float32), xs)
    y = to_time(y)
    mu = jnp.mean(y, axis=-1, keepdims=True)
    var = jnp.mean(jnp.square(y - mu), axis=-1, keepdims=True)
    y = (y - mu) * lax.rsqrt(var + RWKV_LN_EPS) * ln_w.astype(jnp.float32).reshape(H, N) \
        + ln_b.astype(jnp.float32).reshape(H, N)
    y = y + jnp.sum(r_h * k_h * r_k.astype(jnp.float32), axis=-1, keepdims=True) * v_h
    return (y.reshape(B, S, D).astype(h.dtype) * g) @ w_o


def setup_inputs(seed: int = 0) -> dict:
    key = jax.random.key(seed)
    ks = iter(jax.random.split(key, 64))
    D, F = D_MODEL, D_FF
    NA, NB_, NC = N_MLSTM_LAYERS, N_ATTN_LAYERS, N_RWKV_LAYERS

    def nrm(shape, scale):
        return jax.random.normal(next(ks), shape, jnp.float32) * scale

    def gain(shape):
        return 1.0 + nrm(shape, 0.02)

    x = nrm((BATCH, SEQ, D), 1.0)
    positions = jnp.arange(SEQ, dtype=jnp.int32)[None, :] + jax.random.randint(
        next(ks), (BATCH, 1), 0, 4096, dtype=jnp.int32)
    inp = {
        "x": x,
        "positions": positions,
        "ffn1_norm": gain((DEPTH, D)),
        "ffn1_w_gu": nrm((DEPTH, D, 2 * F), D ** -0.5),
        "ffn1_w_down": nrm((DEPTH, F, D), F ** -0.5),
        "mixer_norm": gain((DEPTH, D)),
        "ffn2_norm": gain((DEPTH, D)),
        "ffn2_w_gu": nrm((DEPTH, D, 2 * F), D ** -0.5),
        "ffn2_w_down": nrm((DEPTH, F, D), F ** -0.5),
        "mlstm_w_in": nrm((NA, D, MLSTM_IN_COLS), D ** -0.5),
        "mlstm_b_gate": jnp.stack([nrm((NA, MLSTM_HEADS), 0.1),
                                   jnp.linspace(3.0, 6.0, MLSTM_HEADS)[None, :] + nrm((NA, MLSTM_HEADS), 0.1)], axis=1),
        "mlstm_head_gain": gain((NA, MLSTM_HEADS, MLSTM_DV)),
        "mlstm_w_out": nrm((NA, MLSTM_HEADS * MLSTM_DV, D), (MLSTM_HEADS * MLSTM_DV) ** -0.5),
        "attn_w_qkv": nrm((NB_, D, ATTN_QKV_COLS), D ** -0.5),
        "attn_q_gain": gain((NB_, ATTN_HEAD_DIM)),
        "attn_k_gain": gain((NB_, ATTN_HEAD_DIM)),
        "attn_sinks": nrm((NB_, ATTN_Q_HEADS), 0.5),
        "attn_w_o": nrm((NB_, ATTN_Q_HEADS * ATTN_HEAD_DIM, D), (ATTN_Q_HEADS * ATTN_HEAD_DIM) ** -0.5),
        "rwkv_mix": jax.random.uniform(next(ks), (NC, 6, D), jnp.float32),
        "rwkv_w_rkv": nrm((NC, 3, D, D), D ** -0.5),
        "rwkv_w0": jnp.linspace(-6.5, -1.5, D)[None, :] + nrm((NC, D), 0.1),
        "rwkv_w_lora_a": nrm((NC, D, RWKV_DECAY_LORA), D ** -0.5),
        "rwkv_w_lora_b": nrm((NC, RWKV_DECAY_LORA, D), 0.1 * RWKV_DECAY_LORA ** -0.5),
        "rwkv_a0": nrm((NC, D), 0.1),
        "rwkv_a_lora_a": nrm((NC, D, RWKV_AAA_LORA), D ** -0.5),
        "rwkv_a_lora_b": nrm((NC, RWKV_AAA_LORA, D), RWKV_AAA_LORA ** -0.5),
        "rwkv_g_lora_a": nrm((NC, D, RWKV_GATE_LORA), D ** -0.5),
        "rwkv_g_lora_b": nrm((NC, RWKV_GATE_LORA, D), RWKV_GATE_LORA ** -0.5),
        "rwkv_k_k": 0.85 + nrm((NC, D), 0.02),
        "rwkv_k_a": 1.0 + nrm((NC, D), 0.02),
        "rwkv_r_k": nrm((NC, RWKV_HEADS, RWKV_HEAD), 0.1),
        "rwkv_ln_w": gain((NC, D)),
        "rwkv_ln_b": nrm((NC, D), 0.02),
        "rwkv_w_o": nrm((NC, D, D), D ** -0.5),
    }
    return inp


def reference(x, positions, ffn1_norm, ffn1_w_gu, ffn1_w_down, mixer_norm, ffn2_norm, ffn2_w_gu, ffn2_w_down,
              mlstm_w_in, mlstm_b_gate, mlstm_head_gain, mlstm_w_out,
              attn_w_qkv, attn_q_gain, attn_k_gain, attn_sinks, attn_w_o,
              rwkv_mix, rwkv_w_rkv, rwkv_w0, rwkv_w_lora_a, rwkv_w_lora_b, rwkv_a0, rwkv_a_lora_a, rwkv_a_lora_b,
              rwkv_g_lora_a, rwkv_g_lora_b, rwkv_k_k, rwkv_k_a, rwkv_r_k, rwkv_ln_w, rwkv_ln_b, rwkv_w_o):
    for i in range(DEPTH):
        x = x + 0.5 * swiglu(rmsnorm(x, ffn1_norm[i]), ffn1_w_gu[i], ffn1_w_down[i])
        hn = rmsnorm(x, mixer_norm[i])
        kind, j = i % N_MIXERS, i // N_MIXERS
        if kind == 0:
            y = mlstm_mixer(hn, mlstm_w_in[j], mlstm_b_gate[j], mlstm_head_gain[j], mlstm_w_out[j])
        elif kind == 1:
            y = swa_sink_attention(hn, positions, attn_w_qkv[j], attn_q_gain[j], attn_k_gain[j],
                                   attn_sinks[j], attn_w_o[j])
        else:
            y = rwkv7_mixer(hn, rwkv_mix[j], rwkv_w_rkv[j], rwkv_w0[j], rwkv_w_lora_a[j], rwkv_w_lora_b[j],
                            rwkv_a0[j], rwkv_a_lora_a[j], rwkv_a_lora_b[j], rwkv_g_lora_a[j], rwkv_g_lora_b[j],
                            rwkv_k_k[j], rwkv_k_a[j], rwkv_r_k[j], rwkv_ln_w[j], rwkv_ln_b[j], rwkv_w_o[j])
        x = x + y
        x = x + 0.5 * swiglu(rmsnorm(x, ffn2_norm[i]), ffn2_w_gu[i], ffn2_w_down[i])
    return x
```

```python
import numpy as np
import ml_dtypes
import concourse.bass as bass
import concourse.mybir as mybir
from concourse.bass_utils import run_bass_kernel_spmd

F32 = mybir.dt.float32
BF16 = mybir.dt.bfloat16
I32 = mybir.dt.int32
AF = mybir.ActivationFunctionType
ALU = mybir.AluOpType
AX = mybir.AxisListType

D = 2048
FF = 5632
NCH = D // 128
NJ = FF // 128
TT = 512
EPS = 1e-6


class Tk:
    __slots__ = ("name", "w", "ws", "r", "dsem", "dcnt")

    def __init__(self, name):
        self.name = name
        self.w = None
        self.ws = []
        self.r = {}
        self.dsem = None
        self.dcnt = 0


class KB:
    def __init__(self, nc):
        self.nc = nc
        self.E = {"pe": nc.tensor, "dve": nc.vector, "act": nc.scalar, "pool": nc.gpsimd, "sp": nc.sync}
        self.sem = {k: nc.alloc_semaphore("s_" + k) for k in self.E}
        self.cnt = dict.fromkeys(self.E, 0)
        self.seen = {k: {} for k in self.E}
        self.dsems = []
        self.free = []
        self.uid = 0

    def tk(self, name):
        self.uid += 1
        return Tk(f"{name}_{self.uid}")

    def wait(self, e, tok):
        sem, val, _ = tok
        d = self.seen[e]
        if d.get(sem.num, 0) >= val:
            return
        self.E[e].wait_ge(sem, val)
        d[sem.num] = val

    def deps(self, e, reads, writes, strict=False):
        for t in reads:
            if t.w is not None:
                self.wait(e, t.w)
            for tok in t.ws:
                self.wait(e, tok)
        for t in writes:
            if t.w is not None and (strict or t.w[2] != e):
                self.wait(e, t.w)
            for tok in t.ws:
                self.wait(e, tok)
            for tok in t.r.values():
                if strict or tok[2] != e:
                    self.wait(e, tok)

    def done(self, tok, reads, writes, acc=False):
        for t in writes:
            if acc and t.w is not None:
                t.ws = [x for x in t.ws if x[0].num != t.w[0].num] + [t.w]
            elif not acc:
                t.ws = []
            t.w = tok
            t.r = {}
        for t in reads:
            if t not in writes:
                t.r[tok[0].num] = tok

    def op(self, e, fn, reads=(), writes=()):
        self.deps(e, reads, writes)
        ins = fn(self.E[e])
        self.cnt[e] += 1
        ins.then_inc(self.sem[e], 1)
        self.done((self.sem[e], self.cnt[e], e), reads, writes)

    def mm(self, mms, reads, writes, transpose=False):
        self.deps("pe", reads, writes)
        ins = None
        for m in mms:
            if transpose:
                ins = self.nc.tensor.transpose(m[0], m[1], m[2])
            else:
                ins = self.nc.tensor.matmul(m[0], m[1], m[2], start=m[3], stop=m[4])
        self.cnt["pe"] += 1
        ins.then_inc(self.sem["pe"], 1)
        self.done((self.sem["pe"], self.cnt["pe"], "pe"), reads, writes)

    def dma(self, q, pairs, reads, writes, st, acc=False):
        self.deps(q, reads, writes, strict=True)
        if st.dsem is None:
            if self.free:
                st.dsem, st.dcnt = self.free.pop()
            else:
                st.dsem = self.nc.alloc_semaphore("d_" + st.name)
            self.dsems.append(st)
        for o, i in pairs:
            self.E[q].dma_start(out=o, in_=i).then_inc(st.dsem, 16)
            st.dcnt += 16
        self.done((st.dsem, st.dcnt, "dma"), reads, writes, acc=acc)

    def barrier(self):
        for e in self.E:
            for o in self.E:
                if self.cnt[o] > 0:
                    self.wait(e, (self.sem[o], self.cnt[o], o))
            for st in self.dsems:
                if st.dcnt > 0:
                    self.wait(e, (st.dsem, st.dcnt, "dma"))


SB_LO = 16512
SB_HI = 229344


class Pool:
    def __init__(self, kb):
        self.kb = kb
        self.nc = kb.nc
        self.off = SB_LO
        self.marks = []
        self.tks = []

    def sb(self, name, shape, dt):
        isz = {F32: 4, BF16: 2, I32: 4}[dt]
        n = isz
        for s in shape[1:]:
            n *= s
        n = (n + 31) // 32 * 32
        assert self.off + n <= SB_HI, f"SBUF overflow allocating {name}: {self.off}+{n}"
        self.kb.uid += 1
        t = self.nc.alloc_sbuf_tensor_at(f"{name}_{self.kb.uid}", list(shape), dt, offset=self.off)
        self.off += n
        k = self.kb.tk(name)
        self.tks.append(k)
        return t, k

    def mark(self):
        self.marks.append((self.off, len(self.tks)))

    def release(self):
        self.off, n = self.marks.pop()
        for k in self.tks[n:]:
            if k.dsem is not None:
                self.kb.free.append((k.dsem, k.dcnt))
                self.kb.dsems = [d for d in self.kb.dsems if d is not k]
        self.tks = self.tks[:n]


class G_:
    pass


def setup_globals(nc, consts_ap, params_ap, npar):
    G = G_()
    G.nc = nc
    G.kb = kb = KB(nc)
    G.pool = pool = Pool(kb)
    G.ps = []
    for i in range(8):
        t = nc.alloc_psum_tensor(f"psb{i}", [128, 512], F32)
        G.ps.append((t, kb.tk(f"ps{i}")))
    G.cst, G.cstk = pool.sb("cst", [128, NCONST], F32)
    kb.dma("sp", [(G.cst[:], consts_ap)], reads=[], writes=[G.cstk], st=G.cstk)
    G.par, G.park = pool.sb("par", [128, npar], F32)
    kb.dma("sp", [(G.par[:], params_ap)], reads=[], writes=[G.park], st=G.park)
    G.cb, G.cbk = pool.sb("cstb", [128, NCONST], BF16)
    kb.op("dve", lambda e: e.tensor_copy(out=G.cb[:], in_=G.cst[:]), reads=[G.cstk], writes=[G.cbk])
    G.ident = G.cst[:, C_IDENT:C_IDENT + 128]
    G.ones_b = G.cb[:, C_ONES:C_ONES + 128]
    G.eps = G.cst[:, C_EPS:C_EPS + 1]
    G.one = G.cst[:, C_ONE:C_ONE + 1]
    G.ones_f = G.cst[:, C_ONES:C_ONES + 128]
    G.tri = G.cst[:, C_TRI:C_TRI + 128]
    G.ident_b = G.cb[:, C_IDENT:C_IDENT + 128]
    G.nps = 0
    return G


def newps(G):
    G.nps += 1
    return G.ps[G.nps % 8]


C_IDENT = 0
C_ONES = 128
C_EPS = 256
C_ONE = 257
C_TRI = 264
C_NTRI = 392
NCONST = 520


def make_consts():
    c = np.zeros((128, NCONST), np.float32)
    c[:, C_IDENT:C_IDENT + 128] = np.eye(128, dtype=np.float32)
    c[:, C_ONES:C_ONES + 128] = 1.0
    c[:, C_EPS] = EPS
    c[:, C_ONE] = 1.0
    c[:, C_TRI:C_TRI + 128] = np.triu(np.ones((128, 128), np.float32))
    c[:, C_NTRI:C_NTRI + 128] = 1.0 - np.triu(np.ones((128, 128), np.float32))
    return c


def emit_transpose_in(G, x_ap, dst, dstk, S):
    kb, pool = G.kb, G.pool
    pool.mark()
    xin = [pool.sb("xin", [128, 4, D], F32) for _ in range(2)]
    xo = [pool.sb("xo", [128, NCH, TT], F32) for _ in range(2)]
    xv = x_ap.rearrange("(b p) d -> p b d", p=128)
    dv = dst.rearrange("(c p) s -> p c s", p=128)
    nt = S // TT

    def load(ti):
        t, k = xin[ti % 2]
        kb.dma("sp", [(t[:], xv[:, 4 * ti:4 * ti + 4, :])], reads=[], writes=[k], st=k)

    load(0)
    n = 0
    for ti in range(nt):
        if ti + 1 < nt:
            load(ti + 1)
        I, Ik = xin[ti % 2]
        O, Ok = xo[ti % 2]
        for c in range(NCH):
            p, pk = G.ps[n % 8]
            kb.mm([(p[:, b * 128:(b + 1) * 128], I[:, b, c * 128:(c + 1) * 128], G.ident) for b in range(4)],
                  reads=[Ik, G.cstk], writes=[pk], transpose=True)
            if n % 2 == 0:
                kb.op("dve", lambda e: e.tensor_copy(out=O[:, c, :], in_=p[:]), reads=[pk], writes=[Ok])
            else:
                kb.op("act", lambda e: e.copy(out=O[:, c, :], in_=p[:]), reads=[pk], writes=[Ok])
            n += 1
        kb.dma("pool", [(dv[:, :, ti * TT:(ti + 1) * TT], O[:])], reads=[Ok], writes=[dstk[ti]], st=Ok)
    kb.barrier()
    pool.release()


def emit_transpose_out(G, src, srck, out_ap, S):
    kb, pool = G.kb, G.pool
    pool.mark()
    xin = [pool.sb("yin", [128, NCH, TT], F32) for _ in range(2)]
    xo = [pool.sb("yo", [128, 4, D], F32) for _ in range(2)]
    sv = src.rearrange("(c p) s -> p c s", p=128)
    ov = out_ap.rearrange("(b p) d -> p b d", p=128)
    nt = S // TT
    outk = kb.tk("outdram")

    def load(ti):
        t, k = xin[ti % 2]
        kb.dma("sp", [(t[:], sv[:, :, ti * TT:(ti + 1) * TT])], reads=[srck[ti]], writes=[k], st=k)

    load(0)
    n = 0
    for ti in range(nt):
        if ti + 1 < nt:
            load(ti + 1)
        I, Ik = xin[ti % 2]
        O, Ok = xo[ti % 2]
        for b in range(4):
            for c4 in range(4):
                p, pk = G.ps[n % 8]
                kb.mm([(p[:, q * 128:(q + 1) * 128], I[:, c4 * 4 + q, b * 128:(b + 1) * 128], G.ident) for q in range(4)],
                      reads=[Ik, G.cstk], writes=[pk], transpose=True)
                if n % 2 == 0:
                    kb.op("dve", lambda e: e.tensor_copy(out=O[:, b, c4 * 512:(c4 + 1) * 512], in_=p[:]), reads=[pk], writes=[Ok])
                else:
                    kb.op("act", lambda e: e.copy(out=O[:, b, c4 * 512:(c4 + 1) * 512], in_=p[:]), reads=[pk], writes=[Ok])
                n += 1
        kb.dma("pool", [(ov[:, 4 * ti:4 * ti + 4, :], O[:])], reads=[Ok], writes=[outk], st=Ok)
    kb.barrier()
    pool.release()


def emit_rmsnorm_tile(G, X, Xk, hT, hTk, gcol, sq, rt, rstd, psi=6, T=TT):
    kb = G.kb
    p, pk = G.ps[psi]
    for c in range(NCH):
        s, sk = sq[c % 2]
        kb.op("act", lambda e: e.activation(out=s[:, :T], in_=X[:, c, :], func=AF.Square), reads=[Xk], writes=[sk])
        kb.mm([(p[:, :T], G.ones_b, s[:, :T], c == 0, c == NCH - 1)], reads=[sk, G.cbk], writes=[pk])
    kb.op("act", lambda e: e.activation(out=rt[0][:, :T], in_=p[:, :T], func=AF.Sqrt, scale=1.0 / D, bias=G.eps),
          reads=[pk, G.cstk], writes=[rt[1]])
    kb.op("dve", lambda e: e.reciprocal(out=rstd[0][:, :T], in_=rt[0][:, :T]), reads=[rt[1]], writes=[rstd[1]])
    for c in range(NCH):
        kb.op("dve", lambda e: e.scalar_tensor_tensor(out=hT[:, c, :], in0=X[:, c, :], scalar=G.par[:, gcol + c:gcol + c + 1],
                                                     in1=rstd[0][:, :T], op0=ALU.mult, op1=ALU.mult),
              reads=[Xk, rstd[1], G.park], writes=[hTk])


def emit_ffn(G, src, srck, dst, dstk, gcol, w_gu, w_down, S):
    kb, pool = G.kb, G.pool
    pool.mark()
    xt = [pool.sb("xt", [128, NCH, TT], F32) for _ in range(2)]
    hT, hTk = pool.sb("hT", [128, NCH, TT], BF16)
    actT, actTk = pool.sb("actT", [128, NJ, TT], BF16)
    wgu = [pool.sb("wgu", [128, NCH, 256], BF16) for _ in range(3)]
    wdn = [pool.sb("wdn", [128, NJ, 128], BF16) for _ in range(2)]
    sq = [pool.sb("sq", [128, TT], BF16) for _ in range(2)]
    rstd = pool.sb("rstd", [128, TT], F32)
    rt = pool.sb("rt", [128, TT], F32)
    sil = [pool.sb("sil", [128, TT], F32) for _ in range(2)]
    srcv = src.rearrange("(c p) s -> p c s", p=128)
    dstv = dst.rearrange("(c p) s -> p c s", p=128)
    wguv = w_gu.rearrange("(c p) f -> p c f", p=128)
    wdnv = w_down.rearrange("(j p) d -> p j d", p=128)
    nt = S // TT

    def load(ti):
        t, k = xt[ti % 2]
        kb.dma("sp", [(t[:], srcv[:, :, ti * TT:(ti + 1) * TT])], reads=[srck[ti]], writes=[k], st=k)

    load(0)
    wi = 0
    di = 0
    for ti in range(nt):
        if ti + 1 < nt:
            load(ti + 1)
        X, Xk = xt[ti % 2]
        emit_rmsnorm_tile(G, X, Xk, hT, hTk, gcol, sq, rt, rstd)
        for j in range(NJ):
            W, Wk = wgu[wi % 3]
            wi += 1
            kb.dma("pool", [(W[:, :, 0:128], wguv[:, :, j * 128:(j + 1) * 128]),
                            (W[:, :, 128:256], wguv[:, :, FF + j * 128:FF + (j + 1) * 128])],
                   reads=[], writes=[Wk], st=Wk)
            pg, pgk = G.ps[(2 * j) % 6]
            pu, puk = G.ps[(2 * j + 1) % 6]
            kb.mm([(pg[:], W[:, c, 0:128], hT[:, c, :], c == 0, c == NCH - 1) for c in range(NCH)],
                  reads=[Wk, hTk], writes=[pgk])
            kb.mm([(pu[:], W[:, c, 128:256], hT[:, c, :], c == 0, c == NCH - 1) for c in range(NCH)],
                  reads=[Wk, hTk], writes=[puk])
            s_, s_k = sil[j % 2]
            kb.op("act", lambda e: e.activation(out=s_[:], in_=pg[:], func=AF.Silu), reads=[pgk], writes=[s_k])
            kb.op("dve", lambda e: e.tensor_tensor(out=actT[:, j, :], in0=s_[:], in1=pu[:], op=ALU.mult),
                  reads=[s_k, puk], writes=[actTk])
        for m in range(NCH):
            Wd, Wdk = wdn[di % 2]
            di += 1
            kb.dma("pool", [(Wd[:], wdnv[:, :, m * 128:(m + 1) * 128])], reads=[], writes=[Wdk], st=Wdk)
            pd, pdk = G.ps[6 + m % 2]
            kb.mm([(pd[:], Wd[:, j, :], actT[:, j, :], j == 0, j == NJ - 1) for j in range(NJ)],
                  reads=[Wdk, actTk], writes=[pdk])
            kb.op("dve", lambda e: e.scalar_tensor_tensor(out=X[:, m, :], in0=pd[:], scalar=0.5, in1=X[:, m, :],
                                                         op0=ALU.mult, op1=ALU.add),
                  reads=[pdk, Xk], writes=[Xk])
        kb.dma("sp", [(dstv[:, :, ti * TT:(ti + 1) * TT], X[:])], reads=[Xk], writes=[dstk[ti]], st=Xk)
    kb.barrier()
    pool.release()


MH, MDK, MDV = 8, 128, 256
M_INCOLS = 6160
MP_BG = 0
MP_HG = 16
MP_N = 16 + 2048


def make_mlstm_params(b_gate, head_gain):
    p = np.zeros((128, MP_N), np.float32)
    p[:, 0:8] = b_gate[0][None, :]
    p[:, 8:16] = b_gate[1][None, :]
    p[:, MP_HG:] = head_gain.reshape(1, -1)
    return p


def emit_mlstm(G, src, srck, dst, dstk, gcol, w_in, w_out, mpar_ap, S):
    kb, pool = G.kb, G.pool
    pool.mark()
    xt, xtk = pool.sb("mxt", [128, NCH, TT], F32)
    hT, hTk = pool.sb("mhT", [128, NCH, TT], BF16)
    wb = [pool.sb("mw", [128, NCH, 512], BF16) for _ in range(2)]
    wg, wgk = pool.sb("mwg", [128, NCH, 16], BF16)
    qT, qTk = pool.sb("mqT", [128, MH, TT], BF16)
    kT, kTk = pool.sb("mkT", [128, MH, TT], BF16)
    vaug, vk = pool.sb("mv", [128, 4, MH, 258], BF16)
    so, sok = pool.sb("mso", [128, 4, D], BF16)
    sig = [pool.sb("msig", [128, 512], F32) for _ in range(2)]
    mp, mpk = pool.sb("mpar", [128, MP_N], F32)
    yb = [pool.sb("my", [128, D], BF16) for _ in range(2)]
    kw = [pool.sb("mkw", [128, MH, 128], BF16) for _ in range(2)]
    sT = [pool.sb("msT", [128, MH, 128], BF16) for _ in range(2)]
    am = [pool.sb("mam", [128, MH, 128], F32) for _ in range(2)]
    CT32, CTk = pool.sb("mC32", [128, MH, 258], F32)
    CTb, CTbk = pool.sb("mCb", [128, MH, 258], BF16)
    hn = [pool.sb("mhn", [128, 256], F32) for _ in range(2)]
    junk, junkk = pool.sb("mjunk", [128, 256], F32)
    gza, gzak = pool.sb("mgz", [128, 4, 16], F32)
    gz = [pool.sb("mpbs", [128, 16], F32) for _ in range(2)]
    t1 = [pool.sb("mt1", [128, 8], F32) for _ in range(2)]
    t2 = [pool.sb("mt2", [128, 8], F32) for _ in range(2)]
    av = [pool.sb("mav", [128, 8], F32) for _ in range(2)]
    eb = [pool.sb("meb", [128, 8], F32) for _ in range(2)]
    eg = [pool.sb("meg", [128, 8], F32) for _ in range(2)]
    sm = [pool.sb("msm", [128, 8], F32) for _ in range(4)]
    xres = [pool.sb("mxr", [128, 4, TT], F32) for _ in range(2)]
    sq = [pool.sb("msq", [128, TT], BF16) for _ in range(2)]
    rstd = pool.sb("mrstd", [128, TT], F32)
    rt = pool.sb("mrt", [128, TT], F32)

    srcv = src.rearrange("(c p) s -> p c s", p=128)
    dstv = dst.rearrange("(c p) s -> p c s", p=128)
    winv = w_in.rearrange("(c p) f -> p c f", p=128)
    woutv = w_out.rearrange("(c p) f -> p c f", p=128)
    nt = S // TT

    kb.dma("sp", [(mp[:], mpar_ap)], reads=[], writes=[mpk], st=mpk)
    kb.op("pool", lambda e: e.memset(CT32[:], 0.0), writes=[CTk])
    kb.op("pool", lambda e: e.memset(CTb[:], 0.0), writes=[CTbk])
    kb.op("pool", lambda e: e.memset(vaug[:], 1.0), writes=[vk])
    wcount = [0]

    def wload(view, c0, ncols):
        W, Wk = wb[wcount[0] % 2]
        wcount[0] += 1
        kb.dma("pool", [(W[:, :, 0:ncols], view[:, :, c0:c0 + ncols])], reads=[], writes=[Wk], st=Wk)
        return W, Wk

    ecount = [0]

    def evac(out_ap, in_ap, reads, writes, scale=None):
        ecount[0] += 1
        if ecount[0] % 2 == 0:
            if scale is None:
                kb.op("dve", lambda e: e.tensor_copy(out=out_ap, in_=in_ap), reads=reads, writes=writes)
            else:
                kb.op("dve", lambda e: e.tensor_scalar(out=out_ap, in0=in_ap, scalar1=scale, scalar2=None, op0=ALU.mult),
                      reads=reads, writes=writes)
        else:
            if scale is None:
                kb.op("act", lambda e: e.copy(out=out_ap, in_=in_ap), reads=reads, writes=writes)
            else:
                kb.op("act", lambda e: e.mul(out=out_ap, in_=in_ap, mul=scale), reads=reads, writes=writes)

    for ti in range(nt):
        tsl = slice(ti * TT, (ti + 1) * TT)
        kb.dma("sp", [(xt[:], srcv[:, :, tsl])], reads=[srck[ti]], writes=[xtk], st=xtk)
        emit_rmsnorm_tile(G, xt, xtk, hT, hTk, gcol, sq, rt, rstd)
        for half in range(2):
            W, Wk = wload(winv, half * 512, 512)
            for hh in range(4):
                h = half * 4 + hh
                p, pk = newps(G)
                kb.mm([(p[:], W[:, c, hh * 128:(hh + 1) * 128], hT[:, c, :], c == 0, c == NCH - 1) for c in range(NCH)],
                      reads=[Wk, hTk], writes=[pk])
                evac(qT[:, h, :], p[:], [pk], [qTk])
        for half in range(2):
            W, Wk = wload(winv, 1024 + half * 512, 512)
            for hh in range(4):
                h = half * 4 + hh
                p, pk = newps(G)
                kb.mm([(p[:], W[:, c, hh * 128:(hh + 1) * 128], hT[:, c, :], c == 0, c == NCH - 1) for c in range(NCH)],
                      reads=[Wk, hTk], writes=[pk])
                evac(kT[:, h, :], p[:], [pk], [kTk], scale=MDK ** -0.5)
        for g in range(4):
            W, Wk = wload(winv, 2048 + g * 512, 512)
            for b in range(4):
                p, pk = newps(G)
                kb.mm([(p[:], hT[:, c, b * 128:(b + 1) * 128], W[:, c, :], c == 0, c == NCH - 1) for c in range(NCH)],
                      reads=[Wk, hTk], writes=[pk])
                evac(vaug[:, b, 2 * g:2 * g + 2, 0:256], p[:].rearrange("p (h v) -> p h v", h=2), [pk], [vk])
        for g in range(4):
            W, Wk = wload(winv, 4096 + g * 512, 512)
            for b in range(4):
                p, pk = newps(G)
                kb.mm([(p[:], hT[:, c, b * 128:(b + 1) * 128], W[:, c, :], c == 0, c == NCH - 1) for c in range(NCH)],
                      reads=[Wk, hTk], writes=[pk])
                s_, s_k = sig[(g * 4 + b) % 2]
                kb.op("act", lambda e: e.activation(out=s_[:], in_=p[:], func=AF.Sigmoid), reads=[pk], writes=[s_k])
                kb.op("pool", lambda e: e.tensor_tensor(out=so[:, b, g * 512:(g + 1) * 512], in0=s_[:],
                                                        in1=mp[:, MP_HG + g * 512:MP_HG + (g + 1) * 512], op=ALU.mult),
                      reads=[s_k, mpk], writes=[sok])
        kb.dma("pool", [(wg[:], winv[:, :, 6144:6160])], reads=[], writes=[wgk], st=wgk)
        for b in range(4):
            p, pk = newps(G)
            kb.mm([(p[:, 0:16], hT[:, c, b * 128:(b + 1) * 128], wg[:, c, :], c == 0, c == NCH - 1) for c in range(NCH)],
                  reads=[wgk, hTk], writes=[pk])
            kb.op("dve", lambda e: e.tensor_tensor(out=gza[:, b, :], in0=p[:, 0:16], in1=mp[:, 0:16], op=ALU.add),
                  reads=[pk, mpk], writes=[gzak])
        for b in range(4):
            bs = slice(b * 128, (b + 1) * 128)
            i2 = b % 2
            gz_ = gza[:, b, :]
            gzk = gzak
            t1_, t1k = t1[i2]
            t2_, t2k = t2[i2]
            kb.op("act", lambda e: e.activation(out=t1_[:], in_=gz_[:, 8:16], func=AF.Exp, scale=-1.0), reads=[gzk], writes=[t1k])
            kb.op("act", lambda e: e.activation(out=t2_[:], in_=t1_[:], func=AF.Ln, bias=G.one, scale=1.0),
                  reads=[t1k, G.cstk], writes=[t2k])
            pb, pbk = newps(G)
            kb.mm([(pb[:, 0:8], G.tri, t2_[:], True, True)], reads=[t2k, G.cstk], writes=[pbk])
            kb.mm([(pb[:, 8:16], G.ones_f, t2_[:], True, True)], reads=[t2k, G.cstk], writes=[pbk])
            a_, ak = av[i2]
            eb_, ebk = eb[i2]
            eg_, egk = eg[i2]
            pbs, pbsk = gz[i2]
            kb.op("act", lambda e: e.copy(out=pbs[:], in_=pb[:, 0:16]), reads=[pbk], writes=[pbsk])
            kb.op("dve", lambda e: e.tensor_tensor(out=a_[:], in0=pbs[:, 0:8], in1=gz_[:, 0:8], op=ALU.add),
                  reads=[pbsk, gzk], writes=[ak])
            kb.op("act", lambda e: e.activation(out=a_[:], in_=a_[:], func=AF.Exp), reads=[ak], writes=[ak])
            kb.op("act", lambda e: e.activation(out=eb_[:], in_=pbs[:, 0:8], func=AF.Exp), reads=[pbsk], writes=[ebk])
            kb.op("act", lambda e: e.activation(out=eg_[:], in_=pbs[:, 8:16], func=AF.Exp, scale=-1.0), reads=[pbsk], writes=[egk])
            am_, amk = am[i2]
            kb.op("pool", lambda e: e.tensor_tensor(out=am_[:], in0=G.tri.unsqueeze(1).broadcast_to([128, MH, 128]),
                                                    in1=a_[:].unsqueeze(2).broadcast_to([128, MH, 128]), op=ALU.mult),
                  reads=[ak, G.cstk], writes=[amk])
            pt, ptk = newps(G)
            ptb = pt[:].bitcast(BF16)
            kb.mm([(ptb[:, h * 128:(h + 1) * 128], kT[:, h, bs], G.ident_b) for h in range(MH)],
                  reads=[kTk, G.cbk], writes=[ptk], transpose=True)
            kw_, kwk = kw[i2]
            kb.op("dve", lambda e: e.tensor_tensor(out=kw_[:], in0=ptb.rearrange("p (h d) -> p h d", h=MH),
                                                   in1=a_[:].unsqueeze(2).broadcast_to([128, MH, 128]), op=ALU.mult),
                  reads=[ptk, ak], writes=[kwk])
            sT_, sTk = sT[i2]
            for hb in range(2):
                p, pk = newps(G)
                kb.mm([(p[:, hh * 128:(hh + 1) * 128], kT[:, hb * 4 + hh, bs], qT[:, hb * 4 + hh, bs], True, True) for hh in range(4)],
                      reads=[kTk, qTk], writes=[pk])
                kb.op("dve", lambda e: e.tensor_tensor(out=sT_[:, hb * 4:hb * 4 + 4, :], in0=p[:].rearrange("p (h t) -> p h t", h=4),
                                                       in1=am_[:, hb * 4:hb * 4 + 4, :], op=ALU.mult),
                      reads=[pk, amk], writes=[sTk])
            Y, Yk = yb[i2]
            for h in range(MH):
                p, pk = newps(G)
                kb.mm([(p[:, 0:257], sT_[:, h, :], vaug[:, b, h, 0:257], True, False),
                       (p[:, 0:257], qT[:, h, bs], CTb[:, h, 0:257], False, True)],
                      reads=[sTk, vk, qTk, CTbk], writes=[pk])
                s4, s4k = sm[h % 4]
                kb.op("act", lambda e: e.activation(out=s4[:, 5:6], in_=p[:, 256:257], func=AF.Abs), reads=[pk], writes=[s4k])
                kb.op("dve", lambda e: e.tensor_scalar(out=s4[:, 0:1], in0=s4[:, 5:6], scalar1=eb_[:, h:h + 1], scalar2=None,
                                                       op0=ALU.max), reads=[s4k, ebk], writes=[s4k])
                kb.op("dve", lambda e: e.reciprocal(out=s4[:, 1:2], in_=s4[:, 0:1]), reads=[s4k], writes=[s4k])
                hn_, hnk = hn[h % 2]
                kb.op("act", lambda e: e.activation(out=hn_[:], in_=p[:, 0:256], func=AF.Copy, scale=s4[:, 1:2]),
                      reads=[pk, s4k], writes=[hnk])
                kb.op("act", lambda e: e.activation(out=junk[:], in_=hn_[:], func=AF.Square, accum_out=s4[:, 2:3]),
                      reads=[hnk], writes=[junkk, s4k])
                kb.op("act", lambda e: e.activation(out=s4[:, 3:4], in_=s4[:, 2:3], func=AF.Sqrt, scale=1.0 / MDV, bias=G.eps),
                      reads=[s4k, G.cstk], writes=[s4k])
                kb.op("dve", lambda e: e.reciprocal(out=s4[:, 4:5], in_=s4[:, 3:4]), reads=[s4k], writes=[s4k])
                kb.op("dve", lambda e: e.scalar_tensor_tensor(out=Y[:, h * 256:(h + 1) * 256], in0=hn_[:], scalar=s4[:, 4:5],
                                                             in1=so[:, b, h * 256:(h + 1) * 256], op0=ALU.mult, op1=ALU.mult),
                      reads=[hnk, s4k, sok], writes=[Yk])
                pc, pck = newps(G)
                kb.mm([(pc[:, 0:257], kw_[:, h, :], vaug[:, b, h, 0:257], True, True)], reads=[kwk, vk], writes=[pck])
                kb.op("pool", lambda e: e.tensor_scalar(out=CT32[:, h, :], in0=CT32[:, h, :], scalar1=eg_[:, h:h + 1], scalar2=None,
                                                        op0=ALU.mult), reads=[egk, CTk], writes=[CTk])
                kb.op("dve", lambda e: e.scalar_tensor_tensor(out=CT32[:, h, 0:257], in0=pc[:, 0:257], scalar=eg_[:, h:h + 1],
                                                             in1=CT32[:, h, 0:257], op0=ALU.mult, op1=ALU.add),
                      reads=[pck, egk, CTk], writes=[CTk])
                kb.op("act", lambda e: e.copy(out=CTb[:, h, :], in_=CT32[:, h, :]), reads=[CTk], writes=[CTbk])
            for half in range(2):
                pt, ptk = newps(G)
                ptb = pt[:].bitcast(BF16)
                kb.mm([(ptb[:, q * 128:(q + 1) * 128], Y[:, (half * 8 + q) * 128:(half * 8 + q + 1) * 128], G.ident_b) for q in range(8)],
                      reads=[Yk, G.cbk], writes=[ptk], transpose=True)
                evac(hT[:, half * 8:half * 8 + 8, bs], ptb.rearrange("p (c t) -> p c t", c=8), [ptk], [hTk])
        for mg in range(4):
            W, Wk = wload(woutv, mg * 512, 512)
            XR, XRk = xres[mg % 2]
            kb.dma("sp", [(XR[:], srcv[:, mg * 4:mg * 4 + 4, tsl])], reads=[srck[ti]], writes=[XRk], st=XRk)
            for m4 in range(4):
                p, pk = newps(G)
                kb.mm([(p[:], W[:, c, m4 * 128:(m4 + 1) * 128], hT[:, c, :], c == 0, c == NCH - 1) for c in range(NCH)],
                      reads=[Wk, hTk], writes=[pk])
                kb.op("dve", lambda e: e.tensor_tensor(out=XR[:, m4, :], in0=p[:], in1=XR[:, m4, :], op=ALU.add),
                      reads=[pk, XRk], writes=[XRk])
            kb.dma("sp", [(dstv[:, mg * 4:mg * 4 + 4, tsl], XR[:])], reads=[XRk], writes=[dstk[ti]], st=XRk, acc=(mg > 0))
    kb.barrier()
    pool.release()


AHQ, AHKV, ADH = 32, 4, 64
A_QKV = 2560
AP_QG, AP_KG, AP_INVF = 0, 1, 2
AP_SINK = 8
AP_ROT = 24
AP_BD = 152
AP_N = 280
ROPE_THETA = 500000.0
TWO_PI = 2.0 * np.pi
CW_HI = 6.28125
CW_LO = TWO_PI - 6.28125


def make_attn_params(q_gain, k_gain, sinks):
    p = np.zeros((128, AP_N), np.float32)
    idx = np.arange(128) % 64
    p[:, AP_QG] = q_gain[idx]
    p[:, AP_KG] = k_gain[idx]
    inv_freq = (ROPE_THETA ** (-np.arange(0, 16, 2, dtype=np.float32) / 16)).astype(np.float32)
    p[:, AP_INVF] = np.where(idx < 16, inv_freq[idx % 8], 0.0)
    for par in range(2):
        for g in range(4):
            for slot in range(4):
                p[par * 64:(par + 1) * 64, AP_SINK + 4 * g + slot] = sinks[8 * g + 2 * slot + par]
    for blk in range(2):
        o = blk * 64
        for d in range(8):
            p[o + d + 8, AP_ROT + o + d] = -1.0
            p[o + d, AP_ROT + o + d + 8] = 1.0
        p[o:o + 64, AP_BD + o:AP_BD + o + 64] = 1.0
    return p


def emit_attn(G, src, srck, dst, dstk, gcol, w_qkv, w_o, apar_ap, posb_ap, S):
    kb, pool = G.kb, G.pool
    pool.mark()
    xt, xtk = pool.sb("axt", [128, NCH, TT], F32)
    hT, hTk = pool.sb("ahT", [128, NCH, TT], BF16)
    wb = [pool.sb("aw", [128, NCH, 512], BF16) for _ in range(2)]
    qT, qTk = pool.sb("aqT", [128, NCH, TT], BF16)
    kTd, kTk = pool.sb("akT", [128, AHKV, 128 + TT], BF16)
    vh, vhk = pool.sb("av", [128, 5, 256], BF16)
    apar, apark = pool.sb("apar", [128, AP_N], F32)
    bd, bdk = pool.sb("abd", [128, 128], BF16)
    esink, esk = pool.sb("aes", [128, 16], F32)
    posi, posik = pool.sb("aposi", [128, TT], I32)
    ang, angk = pool.sb("aang", [128, TT], F32)
    r1, r1k = pool.sb("ar1", [128, TT], F32)
    negS, negSk = pool.sb("anS", [128, TT], F32)
    negC, negCk = pool.sb("anC", [128, TT], F32)
    sq = [pool.sb("asq", [128, TT], BF16) for _ in range(2)]
    rstd = pool.sb("arstd", [128, TT], F32)
    rt = pool.sb("art", [128, TT], F32)
    qn = [pool.sb("aqn", [128, TT], F32) for _ in range(2)]
    tu = [pool.sb("atu", [128, TT], F32) for _ in range(4)]
    pT = [pool.sb("apT", [128, 2, 2, 512], BF16) for _ in range(2)]
    dd = [pool.sb("add", [128, 512], F32) for _ in range(2)]
    xres = [pool.sb("axr", [128, 4, TT], F32) for _ in range(2)]
    mpi, mpik = pool.sb("ampi", [128, 1], F32)

    srcv = src.rearrange("(c p) s -> p c s", p=128)
    dstv = dst.rearrange("(c p) s -> p c s", p=128)
    wv = w_qkv.rearrange("(c p) f -> p c f", p=128)
    wov = w_o.rearrange("(c p) f -> p c f", p=128)
    nt = S // TT
    tri_b = G.cb[:, C_TRI:C_TRI + 128]
    ntri_b = G.cb[:, C_NTRI:C_NTRI + 128]
    ones_b64 = G.cb[:, C_ONES:C_ONES + 64]

    kb.dma("sp", [(apar[:], apar_ap)], reads=[], writes=[apark], st=apark)
    kb.op("dve", lambda e: e.tensor_copy(out=bd[:], in_=apar[:, AP_BD:AP_BD + 128]), reads=[apark], writes=[bdk])
    kb.op("act", lambda e: e.activation(out=esink[:], in_=apar[:, AP_SINK:AP_SINK + 16], func=AF.Exp), reads=[apark], writes=[esk])
    kb.op("pool", lambda e: e.memset(mpi[:], -float(np.pi)), writes=[mpik])
    wcount = [0]

    def wload(view, cols):
        W, Wk = wb[wcount[0] % 2]
        wcount[0] += 1
        kb.dma("pool", [(W[:, :, d0:d0 + n], view[:, :, s0:s0 + n]) for d0, s0, n in cols], reads=[], writes=[Wk], st=Wk)
        return W, Wk

    ecount = [0]

    def qk_finish(p, pk, gain_col, out_ap, outk):
        i = ecount[0] % 2
        ecount[0] += 1
        s, sk = sq[i]
        kb.op("act", lambda e: e.activation(out=s[:], in_=p[:], func=AF.Square), reads=[pk], writes=[sk])
        p2, p2k = newps(G)
        kb.mm([(p2[:], bd[:], s[:], True, True)], reads=[sk, bdk], writes=[p2k])
        kb.op("act", lambda e: e.activation(out=rt[0][:], in_=p2[:], func=AF.Sqrt, scale=1.0 / ADH, bias=G.eps),
              reads=[p2k, G.cstk], writes=[rt[1]])
        kb.op("dve", lambda e: e.reciprocal(out=rstd[0][:], in_=rt[0][:]), reads=[rt[1]], writes=[rstd[1]])
        q_, qk_ = qn[i]
        kb.op("dve", lambda e: e.scalar_tensor_tensor(out=q_[:], in0=p[:], scalar=apar[:, gain_col:gain_col + 1], in1=rstd[0][:],
                                                     op0=ALU.mult, op1=ALU.mult), reads=[pk, rstd[1], apark], writes=[qk_])
        p3, p3k = newps(G)
        kb.mm([(p3[:], apar[:, AP_ROT:AP_ROT + 128], q_[:], True, True)], reads=[qk_, apark], writes=[p3k])
        t_, tk_ = tu[2 * i]
        u_, uk_ = tu[2 * i + 1]
        kb.op("pool", lambda e: e.tensor_tensor(out=t_[:], in0=q_[:], in1=negC[:], op=ALU.mult), reads=[qk_, negCk], writes=[tk_])
        kb.op("dve", lambda e: e.tensor_tensor(out=u_[:], in0=p3[:], in1=negS[:], op=ALU.mult), reads=[p3k, negSk], writes=[uk_])
        kb.op("pool", lambda e: e.tensor_tensor(out=out_ap, in0=t_[:], in1=u_[:], op=ALU.add), reads=[tk_, uk_], writes=[outk])

    for ti in range(nt):
        tsl = slice(ti * TT, (ti + 1) * TT)
        kb.dma("sp", [(xt[:], srcv[:, :, tsl])], reads=[srck[ti]], writes=[xtk], st=xtk)
        kb.dma("sp", [(posi[:], posb_ap[:, tsl])], reads=[], writes=[posik], st=posik)
        emit_rmsnorm_tile(G, xt, xtk, hT, hTk, gcol, sq, rt, rstd)
        kb.op("dve", lambda e: e.tensor_copy(out=ang[:], in_=posi[:]), reads=[posik], writes=[angk])
        kb.op("dve", lambda e: e.tensor_scalar(out=ang[:], in0=ang[:], scalar1=apar[:, AP_INVF:AP_INVF + 1], scalar2=None, op0=ALU.mult),
              reads=[angk, apark], writes=[angk])
        for tab, tabk, shift in ((negS, negSk, 0.0), (negC, negCk, 0.25)):
            kb.op("dve", lambda e: e.tensor_scalar(out=r1[:], in0=ang[:], scalar1=1.0 / TWO_PI, scalar2=shift, op0=ALU.mult, op1=ALU.add),
                  reads=[angk], writes=[r1k])
            kb.op("dve", lambda e: e.tensor_copy(out=posi[:], in_=r1[:]), reads=[r1k], writes=[posik])
            kb.op("dve", lambda e: e.tensor_copy(out=r1[:], in_=posi[:]), reads=[posik], writes=[r1k])
            kb.op("dve", lambda e: e.scalar_tensor_tensor(out=tab[:], in0=r1[:], scalar=-CW_HI, in1=ang[:], op0=ALU.mult, op1=ALU.add),
                  reads=[r1k, angk], writes=[tabk])
            kb.op("dve", lambda e: e.scalar_tensor_tensor(out=tab[:], in0=r1[:], scalar=-CW_LO, in1=tab[:], op0=ALU.mult, op1=ALU.add),
                  reads=[r1k, tabk], writes=[tabk])
            if shift:
                kb.op("dve", lambda e: e.tensor_scalar(out=tab[:], in0=tab[:], scalar1=float(np.pi / 2), scalar2=None, op0=ALU.add),
                      reads=[tabk], writes=[tabk])
            kb.op("dve", lambda e: e.tensor_scalar(out=r1[:], in0=tab[:], scalar1=float(np.pi), scalar2=TWO_PI, op0=ALU.is_gt, op1=ALU.mult),
                  reads=[tabk], writes=[r1k])
            kb.op("dve", lambda e: e.tensor_tensor(out=tab[:], in0=tab[:], in1=r1[:], op=ALU.subtract), reads=[tabk, r1k], writes=[tabk])
            kb.op("dve", lambda e: e.tensor_scalar(out=r1[:], in0=tab[:], scalar1=-float(np.pi), scalar2=TWO_PI, op0=ALU.is_lt, op1=ALU.mult),
                  reads=[tabk], writes=[r1k])
            kb.op("dve", lambda e: e.tensor_tensor(out=tab[:], in0=tab[:], in1=r1[:], op=ALU.add), reads=[tabk, r1k], writes=[tabk])
            kb.op("act", lambda e: e.activation(out=tab[:], in_=tab[:], func=AF.Sin), reads=[tabk], writes=[tabk])
        for kg in range(AHKV):
            W, Wk = wload(wv, [(0, 2048 + kg * 64, 64), (64, 2048 + kg * 64, 64)])
            p, pk = newps(G)
            kb.mm([(p[:], W[:, c, 0:128], hT[:, c, :], c == 0, c == NCH - 1) for c in range(NCH)], reads=[Wk, hTk], writes=[pk])
            qk_finish(p, pk, AP_KG, kTd[:, kg, 128:128 + TT], kTk)
        W, Wk = wload(wv, [(0, 2304, 256)])
        for b in range(4):
            p, pk = newps(G)
            kb.mm([(p[:, 0:256], hT[:, c, b * 128:(b + 1) * 128], W[:, c, 0:256], c == 0, c == NCH - 1) for c in range(NCH)],
                  reads=[Wk, hTk], writes=[pk])
            kb.op("act", lambda e: e.copy(out=vh[:, 1 + b, :], in_=p[:, 0:256]), reads=[pk], writes=[vhk])
        for qg in range(4):
            W, Wk = wload(wv, [(0, qg * 512, 512)])
            for c4 in range(4):
                ch = qg * 4 + c4
                p, pk = newps(G)
                kb.mm([(p[:], W[:, c, c4 * 128:(c4 + 1) * 128], hT[:, c, :], c == 0, c == NCH - 1) for c in range(NCH)],
                      reads=[Wk, hTk], writes=[pk])
                qk_finish(p, pk, AP_QG, qT[:, ch, :], qTk)
        for b in range(4):
            gb = ti * 4 + b
            kbs = [kk for kk in (0, 1) if not (kk == 0 and gb == 0)]
            for g in range(AHKV):
                P_, Pk = pT[(b * 4 + g) % 2]
                for kk in kbs:
                    ksl = slice(b * 128 + kk * 128, b * 128 + kk * 128 + 128)
                    for par in range(2):
                        ps_, psk = newps(G)
                        prt = slice(par * 64, par * 64 + 64)
                        kb.mm([(ps_[:, sl * 128:(sl + 1) * 128], kTd[prt, g, ksl], qT[prt, 4 * g + sl, b * 128:(b + 1) * 128], True, True)
                               for sl in range(4)], reads=[kTk, qTk], writes=[psk])
                        kb.op("act", lambda e: e.activation(out=P_[:, kk, par, :], in_=ps_[:], func=AF.Exp, scale=ADH ** -0.5),
                              reads=[psk], writes=[Pk])
                        msk = tri_b if kk == 1 else ntri_b
                        kb.op("dve", lambda e: e.tensor_tensor(out=P_[:, kk, par, :].rearrange("p (h q) -> p h q", h=4),
                                                               in0=P_[:, kk, par, :].rearrange("p (h q) -> p h q", h=4),
                                                               in1=msk.unsqueeze(1).broadcast_to([128, 4, 128]), op=ALU.mult),
                              reads=[Pk, G.cbk], writes=[Pk])
                po, pok = newps(G)
                pd, pdk = newps(G)
                for par in range(2):
                    prt = slice(par * 64, par * 64 + 64)
                    kb.mm([(po[prt, :], vh[:, b + kk, g * 64:(g + 1) * 64], P_[:, kk, par, :], kk == kbs[0], kk == kbs[-1]) for kk in kbs],
                          reads=[vhk, Pk], writes=[pok])
                    kb.mm([(pd[prt, :], ones_b64, P_[:, kk, par, :], kk == kbs[0], kk == kbs[-1]) for kk in kbs],
                          reads=[Pk, G.cbk], writes=[pdk])
                d_, dk_ = dd[g % 2]
                kb.op("dve", lambda e: e.tensor_tensor(out=d_[:].rearrange("p (h q) -> p h q", h=4),
                                                       in0=pd[:].rearrange("p (h q) -> p h q", h=4),
                                                       in1=esink[:, 4 * g:4 * g + 4].unsqueeze(2).broadcast_to([128, 4, 128]), op=ALU.add),
                      reads=[pdk, esk], writes=[dk_])
                kb.op("dve", lambda e: e.reciprocal(out=d_[:], in_=d_[:]), reads=[dk_], writes=[dk_])
                kb.op("dve", lambda e: e.tensor_tensor(out=hT[:, 4 * g:4 * g + 4, b * 128:(b + 1) * 128],
                                                       in0=po[:].rearrange("p (h q) -> p h q", h=4),
                                                       in1=d_[:].rearrange("p (h q) -> p h q", h=4), op=ALU.mult),
                      reads=[pok, dk_], writes=[hTk])
        if ti + 1 < nt:
            kb.op("pool", lambda e: e.tensor_copy(out=kTd[:, :, 0:128], in_=kTd[:, :, TT:TT + 128]), reads=[kTk], writes=[kTk])
            kb.op("pool", lambda e: e.tensor_copy(out=vh[:, 0, :], in_=vh[:, 4, :]), reads=[vhk], writes=[vhk])
        for mg in range(4):
            W, Wk = wload(wov, [(0, mg * 512, 512)])
            XR, XRk = xres[mg % 2]
            kb.dma("sp", [(XR[:], srcv[:, mg * 4:mg * 4 + 4, tsl])], reads=[srck[ti]], writes=[XRk], st=XRk)
            for m4 in range(4):
                p, pk = newps(G)
                kb.mm([(p[:], W[:, c, m4 * 128:(m4 + 1) * 128], hT[:, c, :], c == 0, c == NCH - 1) for c in range(NCH)],
                      reads=[Wk, hTk], writes=[pk])
                kb.op("dve", lambda e: e.tensor_tensor(out=XR[:, m4, :], in0=p[:], in1=XR[:, m4, :], op=ALU.add),
                      reads=[pk, XRk], writes=[XRk])
            kb.dma("sp", [(dstv[:, mg * 4:mg * 4 + 4, tsl], XR[:])], reads=[XRk], writes=[dstk[ti]], st=XRk, acc=(mg > 0))
    kb.barrier()
    pool.release()


def test_attn(nc, G, P, Rf, ins, xA, kA, xB, kB, S):
    w_qkv = nc.dram_tensor("w_qkv", [D, A_QKV], F32, kind="ExternalInput").ap()
    w_o = nc.dram_tensor("w_o", [D, D], F32, kind="ExternalInput").ap()
    apar = nc.dram_tensor("apar", [128, AP_N], F32, kind="ExternalInput").ap()
    posb = nc.dram_tensor("posb", [128, S], I32, kind="ExternalInput").ap()
    ins.update(w_qkv=P["attn_w_qkv"][0], w_o=P["attn_w_o"][0],
               apar=make_attn_params(P["attn_q_gain"][0], P["attn_k_gain"][0], P["attn_sinks"][0]),
               posb=np.ascontiguousarray(np.broadcast_to(Rf["pos"][None, :].astype(np.int32), (128, S))))
    emit_attn(G, xA, kA, xB, kB, 0, w_qkv, w_o, apar, posb, S)


RH, RN = 32, 64
RC = 64
RP_MIX, RP_W0, RP_A0, RP_KK, RP_KA, RP_RK, RP_LNW, RP_LNB = 0, 96, 112, 128, 144, 160, 176, 192
RP_N = 208
RK_RM = 0
RK_SL = 512
RK_SU = 640
RK_IU = 768
RK_BD = 896
RK_ID = 1024
RK_N = 1152
RWKV_LN_EPS = 64e-5
EM05 = float(np.exp(-0.5))


def make_rwkv_params(mix, w0, a0, k_k, k_a, r_k, ln_w, ln_b):
    p = np.zeros((128, RP_N), np.float32)

    def lay(v):
        return np.ascontiguousarray(v.reshape(16, 128).T)

    for i in range(6):
        p[:, RP_MIX + 16 * i:RP_MIX + 16 * (i + 1)] = lay(mix[i])
    p[:, RP_W0:RP_W0 + 16] = lay(w0)
    p[:, RP_A0:RP_A0 + 16] = lay(a0)
    p[:, RP_KK:RP_KK + 16] = lay(k_k)
    p[:, RP_KA:RP_KA + 16] = lay(k_a)
    p[:, RP_RK:RP_RK + 16] = lay(r_k.reshape(-1))
    p[:, RP_LNW:RP_LNW + 16] = lay(ln_w)
    p[:, RP_LNB:RP_LNB + 16] = lay(ln_b)
    return p


def make_rwkv_consts():
    c = np.zeros((128, RK_N), np.float32)
    c[:, RK_RM:RK_RM + 512] = (np.arange(512) % RC != 0).astype(np.float32)[None, :]
    pi = np.arange(128)[:, None]
    fi = np.arange(128)[None, :]
    same = (pi // 64) == (fi // 64)
    c[:, RK_SL:RK_SL + 128] = (same & (pi % 64 > fi % 64))
    c[:, RK_SU:RK_SU + 128] = (same & (pi % 64 < fi % 64))
    c[:, RK_IU:RK_IU + 128] = (same & (pi % 64 <= fi % 64))
    c[:, RK_BD:RK_BD + 128] = same
    c[:, RK_ID:RK_ID + 128] = np.eye(128)
    return c


def emit_rwkv_stage1(G, src, srck, gcol, W, rpar_ap, rk_ap, scr, scrk, S):
    kb, pool = G.kb, G.pool
    pool.mark()
    xt, xtk = pool.sb("rxt", [128, NCH, TT], F32)
    dx, dxk = pool.sb("rdx", [128, NCH, TT], BF16)
    xm = [pool.sb("rxm", [128, NCH, TT], BF16) for _ in range(2)]
    wb = [pool.sb("rw", [128, NCH, 512], BF16) for _ in range(2)]
    wla, wlak = pool.sb("rwla", [128, NCH, 96], BF16)
    ala, alak = pool.sb("rala", [128, NCH, 96], BF16)
    gla, glak = pool.sb("rgla", [128, NCH, 256], BF16)
    wlb, wlbk = pool.sb("rwlb", [96, D], BF16)
    alb, albk = pool.sb("ralb", [96, D], BF16)
    glb, glbk = pool.sb("rglb", [128, 2, D], BF16)
    tw, twk = pool.sb("rtw", [96, TT], BF16)
    ta, tak = pool.sb("rta", [96, TT], BF16)
    tg, tgk = pool.sb("rtg", [128, 2, TT], BF16)
    rp, rpk = pool.sb("rpar", [128, RP_N], F32)
    rc_, rck = pool.sb("rcst", [128, RK_N], F32)
    bdb, bdbk = pool.sb("rbdb", [128, 128], BF16)
    omka, omkak = pool.sb("romka", [128, 16], F32)
    carry, carryk = pool.sb("rcarry", [128, NCH, 1], F32)
    sq = [pool.sb("rsq", [128, TT], BF16) for _ in range(2)]
    rstd = pool.sb("rrstd", [128, TT], F32)
    rt = pool.sb("rrt", [128, TT], F32)
    NTMP = 16
    tmp = [pool.sb("rtmp", [128, TT], F32) for _ in range(NTMP)]
    gcs, gcsk = pool.sb("rgcs", [128, NCH, TT // RC], F32)

    srcv = src.rearrange("(c p) s -> p c s", p=128)
    nt = S // TT
    Wv = {k: (v.rearrange("(c p) f -> p c f", p=128) if k in ("wr", "wk", "wv", "wla", "ala", "gla") else v) for k, v in W.items()}
    scv = {k: v.rearrange("(c p) s -> p c s", p=128) for k, v in scr.items()}

    kb.dma("sp", [(rp[:], rpar_ap)], reads=[], writes=[rpk], st=rpk)
    kb.dma("sp", [(rc_[:], rk_ap)], reads=[], writes=[rck], st=rck)
    kb.dma("pool", [(wla[:], Wv["wla"])], reads=[], writes=[wlak], st=wlak)
    kb.dma("pool", [(ala[:], Wv["ala"])], reads=[], writes=[alak], st=alak)
    kb.dma("pool", [(gla[:], Wv["gla"])], reads=[], writes=[glak], st=glak)
    kb.dma("pool", [(wlb[:], W["wlb"])], reads=[], writes=[wlbk], st=wlbk)
    kb.dma("pool", [(alb[:], W["alb"])], reads=[], writes=[albk], st=albk)
    kb.dma("pool", [(glb[:], W["glb"].rearrange("(j p) f -> p j f", p=128))], reads=[], writes=[glbk], st=glbk)
    kb.op("dve", lambda e: e.tensor_copy(out=bdb[:], in_=rc_[:, RK_BD:RK_BD + 128]), reads=[rck], writes=[bdbk])
    kb.op("dve", lambda e: e.tensor_scalar(out=omka[:], in0=rp[:, RP_KA:RP_KA + 16], scalar1=-1.0, scalar2=1.0, op0=ALU.mult, op1=ALU.add),
          reads=[rpk], writes=[omkak])
    kb.op("pool", lambda e: e.memset(carry[:], 0.0), writes=[carryk])
    wcount = [0]
    tcount = [0]

    def wload(view, c0):
        Wt, Wk = wb[wcount[0] % 2]
        wcount[0] += 1
        kb.dma("pool", [(Wt[:], view[:, :, c0:c0 + 512])], reads=[], writes=[Wk], st=Wk)
        return Wt, Wk

    def T_():
        tcount[0] += 1
        return tmp[tcount[0] % NTMP]

    def mixin(i, buf):
        X, Xk = xm[buf]
        for c in range(NCH):
            eng = "dve" if c % 2 == 0 else "pool"
            if eng == "dve":
                kb.op("dve", lambda e: e.scalar_tensor_tensor(out=X[:, c, :], in0=dx[:, c, :], scalar=rp[:, RP_MIX + 16 * i + c:RP_MIX + 16 * i + c + 1],
                                                             in1=xt[:, c, :], op0=ALU.mult, op1=ALU.add), reads=[dxk, xtk, rpk], writes=[Xk])
            else:
                t_, tk_ = T_()
                kb.op("pool", lambda e: e.tensor_scalar(out=t_[:], in0=dx[:, c, :], scalar1=rp[:, RP_MIX + 16 * i + c:RP_MIX + 16 * i + c + 1],
                                                        scalar2=None, op0=ALU.mult), reads=[dxk, rpk], writes=[tk_])
                kb.op("pool", lambda e: e.tensor_tensor(out=X[:, c, :], in0=t_[:], in1=xt[:, c, :], op=ALU.add), reads=[tk_, xtk], writes=[Xk])
        return X, Xk

    def store(name, c, tsl, t_, tk_, ti):
        kb.dma("sp", [(scv[name][:, c, tsl], t_[:])], reads=[tk_], writes=[scrk[name][ti]], st=tk_, acc=(c > 0))

    for ti in range(nt):
        tsl = slice(ti * TT, (ti + 1) * TT)
        kb.dma("sp", [(xt[:], srcv[:, :, tsl])], reads=[srck[ti]], writes=[xtk], st=xtk)
        p, pk = newps(G)
        for c in range(NCH):
            s, sk = sq[c % 2]
            kb.op("act", lambda e: e.activation(out=s[:], in_=xt[:, c, :], func=AF.Square), reads=[xtk], writes=[sk])
            kb.mm([(p[:], G.ones_b, s[:], c == 0, c == NCH - 1)], reads=[sk, G.cbk], writes=[pk])
        kb.op("act", lambda e: e.activation(out=rt[0][:], in_=p[:], func=AF.Sqrt, scale=1.0 / D, bias=G.eps), reads=[pk, G.cstk], writes=[rt[1]])
        kb.op("dve", lambda e: e.reciprocal(out=rstd[0][:], in_=rt[0][:]), reads=[rt[1]], writes=[rstd[1]])
        for c in range(NCH):
            kb.op("dve", lambda e: e.scalar_tensor_tensor(out=xt[:, c, :], in0=xt[:, c, :], scalar=G.par[:, gcol + c:gcol + c + 1], in1=rstd[0][:],
                                                         op0=ALU.mult, op1=ALU.mult), reads=[xtk, rstd[1], G.park], writes=[xtk])
        kb.op("pool", lambda e: e.tensor_tensor(out=dx[:, :, 1:TT], in0=xt[:, :, 0:TT - 1], in1=xt[:, :, 1:TT], op=ALU.subtract),
              reads=[xtk], writes=[dxk])
        kb.op("pool", lambda e: e.tensor_tensor(out=dx[:, :, 0:1], in0=carry[:], in1=xt[:, :, 0:1], op=ALU.subtract),
              reads=[xtk, carryk], writes=[dxk])
        kb.op("pool", lambda e: e.tensor_copy(out=carry[:], in_=xt[:, :, TT - 1:TT]), reads=[xtk], writes=[carryk])
        X, Xk = mixin(1, 0)
        p, pk = newps(G)
        kb.mm([(p[0:96, :], wla[:, c, :], X[:, c, :], c == 0, c == NCH - 1) for c in range(NCH)], reads=[wlak, Xk], writes=[pk])
        kb.op("act", lambda e: e.activation(out=tw[:], in_=p[0:96, :], func=AF.Tanh), reads=[pk], writes=[twk])
        X, Xk = mixin(4, 1)
        p, pk = newps(G)
        kb.mm([(p[0:96, :], ala[:, c, :], X[:, c, :], c == 0, c == NCH - 1) for c in range(NCH)], reads=[alak, Xk], writes=[pk])
        kb.op("act", lambda e: e.copy(out=ta[:], in_=p[0:96, :]), reads=[pk], writes=[tak])
        X, Xk = mixin(5, 0)
        for j in range(2):
            p, pk = newps(G)
            kb.mm([(p[:], gla[:, c, j * 128:(j + 1) * 128], X[:, c, :], c == 0, c == NCH - 1) for c in range(NCH)], reads=[glak, Xk], writes=[pk])
            kb.op("act", lambda e: e.activation(out=tg[:, j, :], in_=p[:], func=AF.Sigmoid), reads=[pk], writes=[tgk])
        for c in range(NCH):
            p, pk = newps(G)
            kb.mm([(p[:], glb[:, j, c * 128:(c + 1) * 128], tg[:, j, :], j == 0, j == 1) for j in range(2)], reads=[glbk, tgk], writes=[pk])
            t_, tk_ = T_()
            kb.op("act", lambda e: e.copy(out=t_[:], in_=p[:]), reads=[pk], writes=[tk_])
            store("g", c, tsl, t_, tk_, ti)
        Xr, Xrk = mixin(0, 1)
        Xk_, Xkk = mixin(2, 0)
        per_c = {}
        for c4 in range(4):
            Wr, Wrk = wload(Wv["wr"], c4 * 512)
            Wk2, Wk2k = wload(Wv["wk"], c4 * 512)
            for cc in range(4):
                c = c4 * 4 + cc
                col = slice(cc * 128, (cc + 1) * 128)
                pr, prk = newps(G)
                kb.mm([(pr[:], Wr[:, k, col], Xr[:, k, :], k == 0, k == NCH - 1) for k in range(NCH)], reads=[Wrk, Xrk], writes=[prk])
                pkk, pkkk = newps(G)
                kb.mm([(pkk[:], Wk2[:, k, col], Xk_[:, k, :], k == 0, k == NCH - 1) for k in range(NCH)], reads=[Wk2k, Xkk], writes=[pkkk])
                pa, pak = newps(G)
                kb.mm([(pa[:], alb[:, c * 128:(c + 1) * 128], ta[:], True, True)], reads=[albk, tak], writes=[pak])
                pw, pwk = newps(G)
                kb.mm([(pw[:], wlb[:, c * 128:(c + 1) * 128], tw[:], True, True)], reads=[wlbk, twk], writes=[pwk])
                cs = lambda b, c=c: rp[:, b + c:b + c + 1]
                a_, ak = T_()
                kb.op("act", lambda e: e.activation(out=a_[:], in_=pa[:], func=AF.Sigmoid, bias=cs(RP_A0), scale=1.0), reads=[pak, rpk], writes=[ak])
                lw, lwk = T_()
                kb.op("act", lambda e: e.activation(out=lw[:], in_=pw[:], func=AF.Sigmoid, bias=cs(RP_W0), scale=1.0), reads=[pwk, rpk], writes=[lwk])
                kb.op("pool", lambda e: e.tensor_scalar(out=lw[:], in0=lw[:], scalar1=-EM05, scalar2=None, op0=ALU.mult), reads=[lwk], writes=[lwk])
                L, Lk = T_()
                kb.op("dve", lambda e: e.tensor_tensor_scan(out=L[:], data0=rc_[:, RK_RM:RK_RM + TT], data1=lw[:], initial=0.0,
                                                            op0=ALU.mult, op1=ALU.add), reads=[lwk, rck], writes=[Lk])
                Gm, Gmk = T_()
                Gi, Gik = T_()
                Gp, Gpk = T_()
                kb.op("act", lambda e: e.activation(out=Gm[:], in_=L[:], func=AF.Exp), reads=[Lk], writes=[Gmk])
                kb.op("act", lambda e: e.activation(out=Gi[:], in_=L[:], func=AF.Exp, scale=-1.0), reads=[Lk], writes=[Gik])
                kb.op("pool", lambda e: e.tensor_tensor(out=Gp[:], in0=L[:], in1=lw[:], op=ALU.subtract), reads=[Lk, lwk], writes=[Gpk])
                kb.op("act", lambda e: e.activation(out=Gp[:], in_=Gp[:], func=AF.Exp), reads=[Gpk], writes=[Gpk])
                kb.op("pool", lambda e: e.tensor_copy(out=gcs[:, c, :], in_=Gm[:].rearrange("p (j t) -> p j t", t=RC)[:, :, RC - 1]),
                      reads=[Gmk], writes=[gcsk])
                kkr, kkrk = T_()
                kb.op("dve", lambda e: e.tensor_scalar(out=kkr[:], in0=pkk[:], scalar1=cs(RP_KK), scalar2=None, op0=ALU.mult), reads=[pkkk, rpk], writes=[kkrk])
                s, sk = sq[c % 2]
                kb.op("act", lambda e: e.activation(out=s[:], in_=kkr[:], func=AF.Square), reads=[kkrk], writes=[sk])
                pn, pnk = newps(G)
                kb.mm([(pn[:], bdb[:], s[:], True, True)], reads=[sk, bdbk], writes=[pnk])
                nr, nrk = T_()
                kb.op("act", lambda e: e.activation(out=nr[:], in_=pn[:], func=AF.Sqrt), reads=[pnk], writes=[nrk])
                kb.op("dve", lambda e: e.tensor_scalar(out=nr[:], in0=nr[:], scalar1=1e-12, scalar2=None, op0=ALU.max), reads=[nrk], writes=[nrk])
                kb.op("dve", lambda e: e.reciprocal(out=nr[:], in_=nr[:]), reads=[nrk], writes=[nrk])
                kb.op("dve", lambda e: e.tensor_tensor(out=kkr[:], in0=kkr[:], in1=nr[:], op=ALU.mult), reads=[kkrk, nrk], writes=[kkrk])
                km, kmk = T_()
                kb.op("pool", lambda e: e.tensor_scalar(out=km[:], in0=a_[:], scalar1=cs(RP_KA), scalar2=omka[:, c:c + 1], op0=ALU.mult, op1=ALU.add),
                      reads=[ak, rpk, omkak], writes=[kmk])
                kb.op("dve", lambda e: e.tensor_tensor(out=km[:], in0=pkk[:], in1=km[:], op=ALU.mult), reads=[pkkk, kmk], writes=[kmk])
                rr, rrk = T_()
                kb.op("act", lambda e: e.copy(out=rr[:], in_=pr[:]), reads=[prk], writes=[rrk])
                s2, s2k = sq[(c + 1) % 2]
                kb.op("dve", lambda e: e.scalar_tensor_tensor(out=s2[:], in0=rr[:], scalar=cs(RP_RK), in1=km[:], op0=ALU.mult, op1=ALU.mult),
                      reads=[rrk, kmk, rpk], writes=[s2k])
                pb, pbk = newps(G)
                kb.mm([(pb[:], bdb[:], s2[:], True, True)], reads=[s2k, bdbk], writes=[pbk])
                bs_, bsk = T_()
                kb.op("act", lambda e: e.copy(out=bs_[:], in_=pb[:]), reads=[pbk], writes=[bsk])
                store("bs", c, tsl, bs_, bsk, ti)
                kb.op("pool", lambda e: e.tensor_tensor(out=rr[:], in0=rr[:], in1=Gm[:], op=ALU.mult), reads=[rrk, Gmk], writes=[rrk])
                store("rt", c, tsl, rr, rrk, ti)
                kb.op("dve", lambda e: e.tensor_tensor(out=km[:], in0=km[:], in1=Gi[:], op=ALU.mult), reads=[kmk, Gik], writes=[kmk])
                store("kt", c, tsl, km, kmk, ti)
                kb.op("pool", lambda e: e.tensor_tensor(out=a_[:], in0=a_[:], in1=kkr[:], op=ALU.mult), reads=[ak, kkrk], writes=[ak])
                kb.op("pool", lambda e: e.tensor_tensor(out=a_[:], in0=a_[:], in1=Gi[:], op=ALU.mult), reads=[ak, Gik], writes=[ak])
                store("bt", c, tsl, a_, ak, ti)
                kb.op("dve", lambda e: e.scalar_tensor_tensor(out=kkr[:], in0=kkr[:], scalar=-1.0, in1=Gp[:], op0=ALU.mult, op1=ALU.mult),
                      reads=[kkrk, Gpk], writes=[kkrk])
                store("at", c, tsl, kkr, kkrk, ti)
        kb.dma("sp", [(scr["gc"].rearrange("(c p) j -> p c j", p=128)[:, :, ti * (TT // RC):(ti + 1) * (TT // RC)], gcs[:])],
               reads=[gcsk], writes=[scrk["gc"][ti]], st=gcsk)
        Xv, Xvk = mixin(3, 1)
        for c4 in range(4):
            Wv_, Wvk = wload(Wv["wv"], c4 * 512)
            for cc in range(4):
                c = c4 * 4 + cc
                pv, pvk = newps(G)
                kb.mm([(pv[:], Wv_[:, k, cc * 128:(cc + 1) * 128], Xv[:, k, :], k == 0, k == NCH - 1) for k in range(NCH)],
                      reads=[Wvk, Xvk], writes=[pvk])
                t_, tk_ = T_()
                kb.op("act", lambda e: e.copy(out=t_[:], in_=pv[:]), reads=[pvk], writes=[tk_])
                store("v", c, tsl, t_, tk_, ti)
    kb.barrier()
    pool.release()


def emit_rwkv_stage2(G, rk_ap, scr, scrk, S):
    kb, pool = G.kb, G.pool
    pool.mark()
    NG = 4
    rc_, rck = pool.sb("r2cst", [128, RK_N], F32)
    kb.dma("sp", [(rc_[:], rk_ap)], reads=[], writes=[rck], st=rck)
    ident = rc_[:, RK_ID:RK_ID + 128]
    nchunk = S // RC

    def bdtile(name, n=2):
        ts = []
        for _ in range(n):
            t, k = pool.sb(name, [128, 4, 128], F32)
            kb.op("pool", lambda e: e.memset(t[:], 0.0), writes=[k])
            ts.append((t, k))
        return ts

    A_ = bdtile("r2a")
    B_ = bdtile("r2b")
    K_ = bdtile("r2k")
    R_ = bdtile("r2r")
    V_ = bdtile("r2v")
    plain = lambda name, n=2: [pool.sb(name, [128, 4, 128], F32) for _ in range(n)]
    Bt, Kt, Vt = plain("r2bt"), plain("r2kt"), plain("r2vt")
    Pm, Qm = plain("r2P", 4), plain("r2Q", 4)
    Zm = plain("r2Z", 4)
    Mak, Mrb, Mrk = plain("r2mak"), plain("r2mrb"), plain("r2mrk")
    RHS, U = plain("r2rhs"), plain("r2u")
    Yt = [pool.sb("r2y", [128, 4, RC], F32) for _ in range(2)]
    tmpS = plain("r2ts")
    ST = []
    for g in range(NG):
        t, k = pool.sb("r2ST", [128, 4, 128], F32)
        kb.op("pool", lambda e: e.memset(t[:], 0.0), writes=[k])
        ST.append((t, k))
    gc = []
    gcv = scr["gc"].rearrange("(q p) j -> p q j", p=128)
    for g in range(NG):
        t, k = pool.sb("r2gc", [128, 4, nchunk], F32)
        kb.dma("sp", [(t[:], gcv[:, 4 * g:4 * g + 4, :])], reads=list(scrk["gc"]), writes=[k], st=k)
        gc.append((t, k))
    scv = {k: v.rearrange("(q p) s -> p q s", p=128) for k, v in scr.items() if k != "gc"}
    ev = [0]

    def mask_evac(out_t, ps, mcol):
        o, ok = out_t
        p, pk = ps
        kb.op("dve", lambda e: e.tensor_tensor(out=o[:], in0=p[:].rearrange("p (q f) -> p q f", q=4),
                                               in1=rc_[:, mcol:mcol + 128].unsqueeze(1).broadcast_to([128, 4, 128]), op=ALU.mult),
              reads=[pk, rck], writes=[ok])

    def copy_evac(out_t, ps):
        o, ok = out_t
        p, pk = ps
        ev[0] += 1
        if ev[0] % 2:
            kb.op("act", lambda e: e.copy(out=o[:], in_=p[:].rearrange("p (q f) -> p q f", q=4)), reads=[pk], writes=[ok])
        else:
            kb.op("dve", lambda e: e.tensor_copy(out=o[:], in_=p[:].rearrange("p (q f) -> p q f", q=4)), reads=[pk], writes=[ok])

    def mm4(lhs, rhs, extra=None):
        ps = newps(G)
        p, pk = ps
        terms = [(lhs, rhs)] + (extra or [])
        reads = []
        for l, r in terms:
            reads += [l[1], r[1]]
        mms = []
        for q in range(4):
            for i, (l, r) in enumerate(terms):
                mms.append((p[:, q * 128:(q + 1) * 128], l[0][:, q, :], r[0][:, q, :], i == 0, i == len(terms) - 1))
        kb.mm(mms, reads=reads, writes=[pk])
        return ps

    it = 0
    for ci in range(nchunk):
        csl = slice(ci * RC, (ci + 1) * RC)
        ti = (ci * RC) // TT
        for g in range(NG):
            i2 = it % 2
            it += 1
            for name, tl in (("at", A_), ("bt", B_), ("kt", K_), ("rt", R_), ("v", V_)):
                t, k = tl[i2]
                kb.dma("sp", [(t[hh * 64:(hh + 1) * 64, :, hh * 64:(hh + 1) * 64], scv[name][hh * 64:(hh + 1) * 64, 4 * g:4 * g + 4, csl]) for hh in range(2)],
                       reads=[scrk[name][ti]], writes=[k], st=k)
            A, B, K, R, V = A_[i2], B_[i2], K_[i2], R_[i2], V_[i2]
            for srcT, dstT in ((B, Bt[i2]), (K, Kt[i2]), (V, Vt[i2])):
                ps = newps(G)
                kb.mm([(ps[0][:, q * 128:(q + 1) * 128], srcT[0][:, q, :], ident) for q in range(4)], reads=[srcT[1], rck], writes=[ps[1]], transpose=True)
                copy_evac(dstT, ps)
            P0, Q0 = Pm[2 * i2], Qm[2 * i2]
            mask_evac(P0, mm4(A, B), RK_SL)
            mask_evac(Q0, mm4(B, A), RK_SU)
            mask_evac(Mak[i2], mm4(K, A), RK_SU)
            mask_evac(Mrb[i2], mm4(B, R), RK_IU)
            mask_evac(Mrk[i2], mm4(K, R), RK_IU)
            Z = Zm[2 * i2]
            kb.op("pool", lambda e: e.tensor_tensor(out=Z[0][:], in0=Q0[0][:], in1=ident.unsqueeze(1).broadcast_to([128, 4, 128]), op=ALU.add),
                  reads=[Q0[1], rck], writes=[Z[1]])
            Pc, Qc = P0, Q0
            for lvl in range(1, 6):
                Pn, Qn = Pm[2 * i2 + (lvl % 2)], Qm[2 * i2 + (lvl % 2)]
                psP = mm4(Qc, Pc)
                psQ = mm4(Pc, Qc) if lvl < 5 else None
                copy_evac(Pn, psP)
                if psQ is not None:
                    copy_evac(Qn, psQ)
                psZ = mm4(Pn, Z)
                Zn = Zm[2 * i2 + (lvl % 2)]
                kb.op("dve", lambda e: e.tensor_tensor(out=Zn[0][:], in0=psZ[0][:].rearrange("p (q f) -> p q f", q=4), in1=Z[0][:], op=ALU.add),
                      reads=[psZ[1], Z[1]], writes=[Zn[1]])
                Z = Zn
                Pc, Qc = Pn, Qn
            Sg = ST[g]
            copy_evac(RHS[i2], mm4(A, Sg, [(Mak[i2], Vt[i2])]))
            copy_evac(U[i2], mm4(Z, RHS[i2]))
            psY = mm4(Sg, R, [(U[i2], Mrb[i2]), (Vt[i2], Mrk[i2])])
            Y, Yk = Yt[i2]
            pY = psY[0][:].rearrange("p (q f) -> p q f", q=4)
            kb.op("act", lambda e: e.copy(out=Y[0:64, :, :], in_=pY[0:64, :, 0:64]), reads=[psY[1]], writes=[Yk])
            kb.op("act", lambda e: e.copy(out=Y[64:128, :, :], in_=pY[64:128, :, 64:128]), reads=[psY[1]], writes=[Yk])
            kb.dma("pool", [(scv["y"][:, 4 * g:4 * g + 4, csl], Y[:])], reads=[Yk], writes=[scrk["y"][ti]], st=Yk, acc=True)
            psS = mm4(Bt[i2], U[i2], [(Kt[i2], Vt[i2])])
            tS = tmpS[i2]
            kb.op("dve", lambda e: e.tensor_tensor(out=tS[0][:], in0=psS[0][:].rearrange("p (q f) -> p q f", q=4), in1=Sg[0][:], op=ALU.add),
                  reads=[psS[1], Sg[1]], writes=[tS[1]])
            kb.op("pool", lambda e: e.tensor_tensor(out=Sg[0][:], in0=tS[0][:],
                                                    in1=gc[g][0][:, :, ci:ci + 1].broadcast_to([128, 4, 128]), op=ALU.mult),
                  reads=[tS[1], gc[g][1]], writes=[Sg[1]])
    kb.barrier()
    pool.release()


def emit_rwkv_stage3(G, src, srck, dst, dstk, w_o, rpar_ap, rk_ap, scr, scrk, S):
    kb, pool = G.kb, G.pool
    pool.mark()
    rp, rpk = pool.sb("r3par", [128, RP_N], F32)
    rc_, rck = pool.sb("r3cst", [128, RK_N], F32)
    kb.dma("sp", [(rp[:], rpar_ap)], reads=[], writes=[rpk], st=rpk)
    kb.dma("sp", [(rc_[:], rk_ap)], reads=[], writes=[rck], st=rck)
    bd = rc_[:, RK_BD:RK_BD + 128]
    lneps, lnepsk = pool.sb("r3eps", [128, 1], F32)
    kb.op("pool", lambda e: e.memset(lneps[:], RWKV_LN_EPS), writes=[lnepsk])
    ins = {n: [pool.sb("r3" + n, [128, 4, TT], F32) for _ in range(2)] for n in ("y", "bs", "v", "g")}
    zT, zTk = pool.sb("r3z", [128, NCH, TT], BF16)
    wb = [pool.sb("r3w", [128, NCH, 512], BF16) for _ in range(2)]
    xres = [pool.sb("r3xr", [128, 4, TT], F32) for _ in range(2)]
    NT = 8
    tmp = [pool.sb("r3t", [128, TT], F32) for _ in range(NT)]
    tc_ = [0]

    def T_():
        tc_[0] += 1
        return tmp[tc_[0] % NT]

    srcv = src.rearrange("(c p) s -> p c s", p=128)
    dstv = dst.rearrange("(c p) s -> p c s", p=128)
    wov = w_o.rearrange("(c p) f -> p c f", p=128)
    scv = {k: v.rearrange("(c p) s -> p c s", p=128) for k, v in scr.items() if k != "gc"}
    nt = S // TT
    it = 0
    wc = 0
    for ti in range(nt):
        tsl = slice(ti * TT, (ti + 1) * TT)
        for c4 in range(4):
            i2 = it % 2
            it += 1
            for n in ("y", "bs", "v", "g"):
                t, k = ins[n][i2]
                kb.dma("sp", [(t[:], scv[n][:, 4 * c4:4 * c4 + 4, tsl])], reads=[scrk[n][ti]], writes=[k], st=k)
            Yt, Ytk = ins["y"][i2]
            Bs, Bsk = ins["bs"][i2]
            Vv, Vvk = ins["v"][i2]
            Gg, Ggk = ins["g"][i2]
            for cc in range(4):
                c = 4 * c4 + cc
                pm, pmk = newps(G)
                kb.mm([(pm[:], bd, Yt[:, cc, :], True, True)], reads=[Ytk, rck], writes=[pmk])
                s, sk = T_()
                kb.op("act", lambda e: e.activation(out=s[:], in_=Yt[:, cc, :], func=AF.Square), reads=[Ytk], writes=[sk])
                pq, pqk = newps(G)
                kb.mm([(pq[:], bd, s[:], True, True)], reads=[sk, rck], writes=[pqk])
                mu, muk = T_()
                kb.op("act", lambda e: e.mul(out=mu[:], in_=pm[:], mul=1.0 / RN), reads=[pmk], writes=[muk])
                m2, m2k = T_()
                kb.op("pool", lambda e: e.tensor_tensor(out=m2[:], in0=mu[:], in1=mu[:], op=ALU.mult), reads=[muk], writes=[m2k])
                kb.op("dve", lambda e: e.scalar_tensor_tensor(out=m2[:], in0=pq[:], scalar=1.0 / RN, in1=m2[:], op0=ALU.mult, op1=ALU.subtract),
                      reads=[pqk, m2k], writes=[m2k])
                kb.op("act", lambda e: e.activation(out=m2[:], in_=m2[:], func=AF.Sqrt, bias=lneps[:, 0:1], scale=1.0), reads=[m2k, lnepsk], writes=[m2k])
                kb.op("dve", lambda e: e.reciprocal(out=m2[:], in_=m2[:]), reads=[m2k], writes=[m2k])
                kb.op("pool", lambda e: e.tensor_tensor(out=mu[:], in0=Yt[:, cc, :], in1=mu[:], op=ALU.subtract), reads=[Ytk, muk], writes=[muk])
                kb.op("dve", lambda e: e.tensor_tensor(out=mu[:], in0=mu[:], in1=m2[:], op=ALU.mult), reads=[muk, m2k], writes=[muk])
                kb.op("dve", lambda e: e.tensor_scalar(out=mu[:], in0=mu[:], scalar1=rp[:, RP_LNW + c:RP_LNW + c + 1], scalar2=rp[:, RP_LNB + c:RP_LNB + c + 1],
                                                       op0=ALU.mult, op1=ALU.add), reads=[muk, rpk], writes=[muk])
                bn, bnk = T_()
                kb.op("pool", lambda e: e.tensor_tensor(out=bn[:], in0=Bs[:, cc, :], in1=Vv[:, cc, :], op=ALU.mult), reads=[Bsk, Vvk], writes=[bnk])
                kb.op("pool", lambda e: e.tensor_tensor(out=mu[:], in0=mu[:], in1=bn[:], op=ALU.add), reads=[muk, bnk], writes=[muk])
                kb.op("dve", lambda e: e.tensor_tensor(out=zT[:, c, :], in0=mu[:], in1=Gg[:, cc, :], op=ALU.mult), reads=[muk, Ggk], writes=[zTk])
        for mg in range(4):
            W, Wk = wb[wc % 2]
            wc += 1
            kb.dma("pool", [(W[:], wov[:, :, mg * 512:(mg + 1) * 512])], reads=[], writes=[Wk], st=Wk)
            XR, XRk = xres[mg % 2]
            kb.dma("sp", [(XR[:], srcv[:, mg * 4:mg * 4 + 4, tsl])], reads=[srck[ti]], writes=[XRk], st=XRk)
            for m4 in range(4):
                p, pk = newps(G)
                kb.mm([(p[:], W[:, c, m4 * 128:(m4 + 1) * 128], zT[:, c, :], c == 0, c == NCH - 1) for c in range(NCH)],
                      reads=[Wk, zTk], writes=[pk])
                kb.op("dve", lambda e: e.tensor_tensor(out=XR[:, m4, :], in0=p[:], in1=XR[:, m4, :], op=ALU.add),
                      reads=[pk, XRk], writes=[XRk])
            kb.dma("sp", [(dstv[:, mg * 4:mg * 4 + 4, tsl], XR[:])], reads=[XRk], writes=[dstk[ti]], st=XRk, acc=(mg > 0))
    kb.barrier()
    pool.release()


RW_SCR = ("at", "bt", "kt", "rt", "v", "bs", "g", "y")


def emit_rwkv(G, nc, src, srck, dst, dstk, gcol, W, rpar_ap, rk_ap, S, tag="r"):
    scr = {n: nc.dram_tensor(f"{tag}_{n}", [D, S], F32, kind="Internal").ap() for n in RW_SCR}
    scr["gc"] = nc.dram_tensor(f"{tag}_gc", [D, S // RC], F32, kind="Internal").ap()
    scrk = {n: [G.kb.tk(f"{tag}{n}") for _ in range(S // TT)] for n in scr}
    emit_rwkv_stage1(G, src, srck, gcol, W, rpar_ap, rk_ap, {k: v for k, v in scr.items() if k != "y"}, scrk, S)
    emit_rwkv_stage2(G, rk_ap, scr, scrk, S)
    emit_rwkv_stage3(G, src, srck, dst, dstk, W["wo"], rpar_ap, rk_ap, scr, scrk, S)


def test_rwkv(nc, G, P, Rf, ins, xA, kA, xB, kB, S):
    W = {}
    def din(name, arr):
        arr = np.ascontiguousarray(arr, dtype=np.float32)
        ins[name] = arr
        return nc.dram_tensor(name, list(arr.shape), F32, kind="ExternalInput").ap()
    W["wr"] = din("rw_r", P["rwkv_w_rkv"][0][0])
    W["wk"] = din("rw_k", P["rwkv_w_rkv"][0][1])
    W["wv"] = din("rw_v", P["rwkv_w_rkv"][0][2])
    W["wla"] = din("rw_wla", P["rwkv_w_lora_a"][0])
    W["wlb"] = din("rw_wlb", P["rwkv_w_lora_b"][0])
    W["ala"] = din("rw_ala", P["rwkv_a_lora_a"][0])
    W["alb"] = din("rw_alb", P["rwkv_a_lora_b"][0])
    W["gla"] = din("rw_gla", P["rwkv_g_lora_a"][0])
    W["glb"] = din("rw_glb", P["rwkv_g_lora_b"][0])
    W["wo"] = din("rw_wo", P["rwkv_w_o"][0])
    rpar = din("rw_par", make_rwkv_params(P["rwkv_mix"][0], P["rwkv_w0"][0], P["rwkv_a0"][0], P["rwkv_k_k"][0], P["rwkv_k_a"][0],
                                          P["rwkv_r_k"][0], P["rwkv_ln_w"][0], P["rwkv_ln_b"][0]))
    rk = din("rw_cst", make_rwkv_consts())
    emit_rwkv(G, nc, xA, kA, xB, kB, 0, W, rpar, rk, S)


DEPTH = 4
SEQ = 4096
NCORES = 8
LAUNCH_PLAN = [[0], [('f1', 1), ('mx', 1)], [('f2', 1)], [2], [3]]


def build_module(S=SEQ, depth=DEPTH, layers=None):
    steps = expand_steps(layers if layers is not None else range(depth))
    nc = bass.Bass("TRN2", target_bir_lowering=False)
    dt = lambda name, shape, dtype=F32: nc.dram_tensor(name, list(shape), dtype, kind="ExternalInput").ap()
    x = dt("x", [S, D])
    consts = dt("consts", [128, NCONST])
    params = dt("params", [128, 48 * DEPTH])
    out = nc.dram_tensor("out", [S, D], F32, kind="ExternalOutput").ap()
    bufs = [nc.dram_tensor(n, [D, S], F32, kind="Internal").ap() for n in ("xA", "xB")]
    G = setup_globals(nc, consts, params, 48 * DEPTH)
    nt = S // TT
    ks = [[G.kb.tk(f"x{b}") for _ in range(nt)] for b in range(2)]
    cur = 0
    emit_transpose_in(G, x, bufs[0], ks[0], S)
    for st, l in steps:
        kind, j = l % 3, l // 3
        if st == "f1":
            emit_ffn(G, bufs[cur], ks[cur], bufs[1 - cur], ks[1 - cur], 48 * l, dt(f"ffn1_w_gu_{l}", [D, 2 * FF]), dt(f"ffn1_w_down_{l}", [FF, D]), S)
        elif st == "f2":
            emit_ffn(G, bufs[cur], ks[cur], bufs[1 - cur], ks[1 - cur], 48 * l + 32, dt(f"ffn2_w_gu_{l}", [D, 2 * FF]), dt(f"ffn2_w_down_{l}", [FF, D]), S)
        elif kind == 0:
            emit_mlstm(G, bufs[cur], ks[cur], bufs[1 - cur], ks[1 - cur], 48 * l + 16, dt(f"mlstm_w_in_{j}", [D, M_INCOLS]),
                       dt(f"mlstm_w_out_{j}", [D, D]), dt(f"mpar_{j}", [128, MP_N]), S)
        elif kind == 1:
            emit_attn(G, bufs[cur], ks[cur], bufs[1 - cur], ks[1 - cur], 48 * l + 16, dt("attn_w_qkv", [D, A_QKV]), dt("attn_w_o", [D, D]),
                      dt("apar", [128, AP_N]), dt("posb", [128, S], I32), S)
        else:
            RW = {k: dt("rwkv_" + k, s) for k, s in RW_SHAPES.items()}
            emit_rwkv(G, nc, bufs[cur], ks[cur], bufs[1 - cur], ks[1 - cur], 48 * l + 16, RW, dt("rpar", [128, RP_N]), dt("rcst", [128, RK_N]), S)
        cur = 1 - cur
    emit_transpose_out(G, bufs[cur], ks[cur], out, S)
    G.kb.barrier()
    return nc


def expand_steps(layers):
    steps = []
    for it in layers:
        if isinstance(it, (tuple, list)):
            steps.append((it[0], it[1]))
        else:
            steps += [("f1", it), ("mx", it), ("f2", it)]
    return steps


RW_SHAPES = {"wr": [D, D], "wk": [D, D], "wv": [D, D], "wla": [D, 96], "wlb": [96, D], "ala": [D, 96], "alb": [96, D],
             "gla": [D, 256], "glb": [256, D], "wo": [D, D]}


def step_inputs(inp, st, l):
    f32 = lambda a: np.ascontiguousarray(np.asarray(a), dtype=np.float32)
    m = {}
    kind, j = l % 3, l // 3
    if st in ("f1", "f2"):
        pre = "ffn1" if st == "f1" else "ffn2"
        m[f"{pre}_w_gu_{l}"] = f32(inp[f"{pre}_w_gu"][l])
        m[f"{pre}_w_down_{l}"] = f32(inp[f"{pre}_w_down"][l])
    elif kind == 0:
        m[f"mlstm_w_in_{j}"] = f32(inp["mlstm_w_in"][j])
        m[f"mlstm_w_out_{j}"] = f32(inp["mlstm_w_out"][j])
        m[f"mpar_{j}"] = make_mlstm_params(np.asarray(inp["mlstm_b_gate"][j]), np.asarray(inp["mlstm_head_gain"][j]))
    elif kind == 1:
        m["attn_w_qkv"] = f32(inp["attn_w_qkv"][j])
        m["attn_w_o"] = f32(inp["attn_w_o"][j])
        m["apar"] = make_attn_params(np.asarray(inp["attn_q_gain"][j]), np.asarray(inp["attn_k_gain"][j]), np.asarray(inp["attn_sinks"][j]))
    else:
        rkv = np.asarray(inp["rwkv_w_rkv"][j])
        m["rwkv_wr"], m["rwkv_wk"], m["rwkv_wv"] = f32(rkv[0]), f32(rkv[1]), f32(rkv[2])
        for k, n in (("wla", "rwkv_w_lora_a"), ("wlb", "rwkv_w_lora_b"), ("ala", "rwkv_a_lora_a"), ("alb", "rwkv_a_lora_b"),
                     ("gla", "rwkv_g_lora_a"), ("glb", "rwkv_g_lora_b"), ("wo", "rwkv_w_o")):
            m["rwkv_" + k] = f32(inp[n][j])
        m["rpar"] = make_rwkv_params(*[np.asarray(inp[k][j]) for k in ("rwkv_mix", "rwkv_w0", "rwkv_a0", "rwkv_k_k", "rwkv_k_a", "rwkv_r_k",
                                                                      "rwkv_ln_w", "rwkv_ln_b")])
        m["rcst"] = make_rwkv_consts()
    return m


def kernel(**inp):
    f32 = lambda a: np.ascontiguousarray(np.asarray(a), dtype=np.float32)
    lay = lambda v: np.ascontiguousarray(np.asarray(v, np.float32).reshape(16, 128).T)
    params = np.zeros((128, 48 * DEPTH), np.float32)
    for l in range(DEPTH):
        params[:, 48 * l:48 * l + 16] = lay(inp["ffn1_norm"][l])
        params[:, 48 * l + 16:48 * l + 32] = lay(inp["mixer_norm"][l])
        params[:, 48 * l + 32:48 * l + 48] = lay(inp["ffn2_norm"][l])
    x = np.asarray(inp["x"])
    pos = np.asarray(inp["positions"]).astype(np.int32)
    outs = [f32(x[b]) for b in range(NCORES)]
    for layers in LAUNCH_PLAN:
        shared = {"consts": make_consts(), "params": params}
        steps = expand_steps(layers)
        for st, l in steps:
            shared.update(step_inputs(inp, st, l))
        in_maps = []
        for b in range(NCORES):
            m = dict(shared)
            m["x"] = outs[b]
            if any(st == "mx" and l % 3 == 1 for st, l in steps):
                m["posb"] = np.ascontiguousarray(np.broadcast_to(pos[b][None, :], (128, SEQ)))
            in_maps.append(m)
        nc = build_module(layers=layers)
        res = run_bass_kernel_spmd(nc, in_maps, core_ids=list(range(NCORES)))
        outs = [np.ascontiguousarray(np.asarray(r["out"]), dtype=np.float32) for r in res.results]
    return np.stack(outs).astype(np.float32)
```

```python
import numpy as np
import ml_dtypes
import concourse.bass as bass
import concourse.mybir as mybir
from concourse.bass_utils import run_bass_kernel_spmd

F32 = mybir.dt.float32
BF16 = mybir.dt.bfloat16
I32 = mybir.dt.int32
AF = mybir.ActivationFunctionType
ALU = mybir.AluOpType
AX = mybir.AxisListType

D = 2048
FF = 5632
NCH = D // 128
NJ = FF // 128
TT = 512
EPS = 1e-6


class Tk:
    __slots__ = ("name", "w", "ws", "r", "dsem", "dcnt")

    def __init__(self, name):
        self.name = name
        self.w = None
        self.ws = []
        self.r = {}
        self.dsem = None
        self.dcnt = 0


class KB:
    def __init__(self, nc):
        self.nc = nc
        self.E = {"pe": nc.tensor, "dve": nc.vector, "act": nc.scalar, "pool": nc.gpsimd, "sp": nc.sync}
        self.sem = {k: nc.alloc_semaphore("s_" + k) for k in self.E}
        self.cnt = dict.fromkeys(self.E, 0)
        self.seen = {k: {} for k in self.E}
        self.dsems = []
        self.free = []
        self.inflight = {k: [] for k in self.E}
        self.uid = 0

    def tk(self, name):
        self.uid += 1
        return Tk(f"{name}_{self.uid}")

    def wait(self, e, tok):
        sem, val, _ = tok
        d = self.seen[e]
        if d.get(sem.num, 0) >= val:
            return
        self.E[e].wait_ge(sem, val)
        d[sem.num] = val

    def deps(self, e, reads, writes, strict=False):
        for t in reads:
            if t.w is not None:
                self.wait(e, t.w)
            for tok in t.ws:
                self.wait(e, tok)
        for t in writes:
            if t.w is not None and (strict or t.w[2] != e):
                self.wait(e, t.w)
            for tok in t.ws:
                self.wait(e, tok)
            for tok in t.r.values():
                if strict or tok[2] != e:
                    self.wait(e, tok)

    def done(self, tok, reads, writes, acc=False):
        for t in writes:
            if acc and t.w is not None:
                t.ws = [x for x in t.ws if x[0].num != t.w[0].num] + [t.w]
            elif not acc:
                t.ws = []
            t.w = tok
            t.r = {}
        for t in reads:
            if t not in writes:
                t.r[tok[0].num] = tok

    def op(self, e, fn, reads=(), writes=()):
        self.deps(e, reads, writes)
        ins = fn(self.E[e])
        self.cnt[e] += 1
        ins.then_inc(self.sem[e], 1)
        self.done((self.sem[e], self.cnt[e], e), reads, writes)

    def mm(self, mms, reads, writes, transpose=False):
        self.deps("pe", reads, writes)
        ins = None
        for m in mms:
            if transpose:
                ins = self.nc.tensor.transpose(m[0], m[1], m[2])
            else:
                ins = self.nc.tensor.matmul(m[0], m[1], m[2], start=m[3], stop=m[4])
        self.cnt["pe"] += 1
        ins.then_inc(self.sem["pe"], 1)
        self.done((self.sem["pe"], self.cnt["pe"], "pe"), reads, writes)

    def dma(self, q, pairs, reads, writes, st, acc=False):
        self.deps(q, reads, writes, strict=True)
        if st.dsem is None:
            if self.free:
                st.dsem, st.dcnt = self.free.pop()
            else:
                st.dsem = self.nc.alloc_semaphore("d_" + st.name)
            self.dsems.append(st)
        split = []
        for o, i in pairs:
            so, si = tuple(o.shape), tuple(i.shape)
            if len(so) == 3 and so == si and so[1] > DMA_SPLIT:
                for a in range(0, so[1], DMA_SPLIT):
                    split.append((o[:, a:min(a + DMA_SPLIT, so[1]), :], i[:, a:min(a + DMA_SPLIT, so[1]), :]))
            else:
                split.append((o, i))
        fl = self.inflight[q]
        for o, i in split:
            if len(fl) >= DMA_CAP:
                self.wait(q, fl.pop(0))
            self.E[q].dma_start(out=o, in_=i).then_inc(st.dsem, 16)
            st.dcnt += 16
            fl.append((st.dsem, st.dcnt, "dma"))
        self.done((st.dsem, st.dcnt, "dma"), reads, writes, acc=acc)

    def barrier(self):
        for e in self.E:
            for o in self.E:
                if self.cnt[o] > 0:
                    self.wait(e, (self.sem[o], self.cnt[o], o))
            for st in self.dsems:
                if st.dcnt > 0:
                    self.wait(e, (st.dsem, st.dcnt, "dma"))


DMA_SPLIT = 8
DMA_CAP = 12
SB_LO = 16512
SB_HI = 229344


class Pool:
    def __init__(self, kb):
        self.kb = kb
        self.nc = kb.nc
        self.off = SB_LO
        self.marks = []
        self.tks = []

    def sb(self, name, shape, dt):
        isz = {F32: 4, BF16: 2, I32: 4}[dt]
        n = isz
        for s in shape[1:]:
            n *= s
        n = (n + 31) // 32 * 32
        assert self.off + n <= SB_HI, f"SBUF overflow allocating {name}: {self.off}+{n}"
        self.kb.uid += 1
        t = self.nc.alloc_sbuf_tensor_at(f"{name}_{self.kb.uid}", list(shape), dt, offset=self.off)
        self.off += n
        k = self.kb.tk(name)
        self.tks.append(k)
        return t, k

    def mark(self):
        self.marks.append((self.off, len(self.tks)))

    def release(self):
        self.off, n = self.marks.pop()
        for k in self.tks[n:]:
            if k.dsem is not None:
                self.kb.free.append((k.dsem, k.dcnt))
                self.kb.dsems = [d for d in self.kb.dsems if d is not k]
        self.tks = self.tks[:n]


class G_:
    pass


def setup_globals(nc, consts_ap, params_ap, npar):
    G = G_()
    G.nc = nc
    G.kb = kb = KB(nc)
    G.pool = pool = Pool(kb)
    G.ps = []
    for i in range(8):
        t = nc.alloc_psum_tensor(f"psb{i}", [128, 512], F32)
        G.ps.append((t, kb.tk(f"ps{i}")))
    G.cst, G.cstk = pool.sb("cst", [128, NCONST], F32)
    kb.dma("sp", [(G.cst[:], consts_ap)], reads=[], writes=[G.cstk], st=G.cstk)
    G.par, G.park = pool.sb("par", [128, npar], F32)
    kb.dma("sp", [(G.par[:], params_ap)], reads=[], writes=[G.park], st=G.park)
    G.cb, G.cbk = pool.sb("cstb", [128, NCONST], BF16)
    kb.op("dve", lambda e: e.tensor_copy(out=G.cb[:], in_=G.cst[:]), reads=[G.cstk], writes=[G.cbk])
    G.ident = G.cst[:, C_IDENT:C_IDENT + 128]
    G.ones_b = G.cb[:, C_ONES:C_ONES + 128]
    G.eps = G.cst[:, C_EPS:C_EPS + 1]
    G.one = G.cst[:, C_ONE:C_ONE + 1]
    G.ones_f = G.cst[:, C_ONES:C_ONES + 128]
    G.tri = G.cst[:, C_TRI:C_TRI + 128]
    G.ident_b = G.cb[:, C_IDENT:C_IDENT + 128]
    G.nps = 0
    return G


def newps(G):
    G.nps += 1
    return G.ps[G.nps % 8]


C_IDENT = 0
C_ONES = 128
C_EPS = 256
C_ONE = 257
C_TRI = 264
C_NTRI = 392
NCONST = 520


def make_consts():
    c = np.zeros((128, NCONST), np.float32)
    c[:, C_IDENT:C_IDENT + 128] = np.eye(128, dtype=np.float32)
    c[:, C_ONES:C_ONES + 128] = 1.0
    c[:, C_EPS] = EPS
    c[:, C_ONE] = 1.0
    c[:, C_TRI:C_TRI + 128] = np.triu(np.ones((128, 128), np.float32))
    c[:, C_NTRI:C_NTRI + 128] = 1.0 - np.triu(np.ones((128, 128), np.float32))
    return c


def emit_transpose_in(G, x_ap, dst, dstk, S):
    kb, pool = G.kb, G.pool
    pool.mark()
    xin = [pool.sb("xin", [128, 4, D], F32) for _ in range(2)]
    xo = [pool.sb("xo", [128, NCH, TT], F32) for _ in range(2)]
    xv = x_ap.rearrange("(b p) d -> p b d", p=128)
    dv = dst.rearrange("(c p) s -> p c s", p=128)
    nt = S // TT

    def load(ti):
        t, k = xin[ti % 2]
        kb.dma("sp", [(t[:], xv[:, 4 * ti:4 * ti + 4, :])], reads=[], writes=[k], st=k)

    load(0)
    n = 0
    for ti in range(nt):
        if ti + 1 < nt:
            load(ti + 1)
        I, Ik = xin[ti % 2]
        O, Ok = xo[ti % 2]
        for c in range(NCH):
            p, pk = G.ps[n % 8]
            kb.mm([(p[:, b * 128:(b + 1) * 128], I[:, b, c * 128:(c + 1) * 128], G.ident) for b in range(4)],
                  reads=[Ik, G.cstk], writes=[pk], transpose=True)
            if n % 2 == 0:
                kb.op("dve", lambda e: e.tensor_copy(out=O[:, c, :], in_=p[:]), reads=[pk], writes=[Ok])
            else:
                kb.op("act", lambda e: e.copy(out=O[:, c, :], in_=p[:]), reads=[pk], writes=[Ok])
            n += 1
        kb.dma("pool", [(dv[:, :, ti * TT:(ti + 1) * TT], O[:])], reads=[Ok], writes=[dstk[ti]], st=Ok)
    kb.barrier()
    pool.release()


def emit_transpose_out(G, src, srck, out_ap, S):
    kb, pool = G.kb, G.pool
    pool.mark()
    xin = [pool.sb("yin", [128, NCH, TT], F32) for _ in range(2)]
    xo = [pool.sb("yo", [128, 4, D], F32) for _ in range(2)]
    sv = src.rearrange("(c p) s -> p c s", p=128)
    ov = out_ap.rearrange("(b p) d -> p b d", p=128)
    nt = S // TT
    outk = kb.tk("outdram")

    def load(ti):
        t, k = xin[ti % 2]
        kb.dma("sp", [(t[:], sv[:, :, ti * TT:(ti + 1) * TT])], reads=[srck[ti]], writes=[k], st=k)

    load(0)
    n = 0
    for ti in range(nt):
        if ti + 1 < nt:
            load(ti + 1)
        I, Ik = xin[ti % 2]
        O, Ok = xo[ti % 2]
        for b in range(4):
            for c4 in range(4):
                p, pk = G.ps[n % 8]
                kb.mm([(p[:, q * 128:(q + 1) * 128], I[:, c4 * 4 + q, b * 128:(b + 1) * 128], G.ident) for q in range(4)],
                      reads=[Ik, G.cstk], writes=[pk], transpose=True)
                if n % 2 == 0:
                    kb.op("dve", lambda e: e.tensor_copy(out=O[:, b, c4 * 512:(c4 + 1) * 512], in_=p[:]), reads=[pk], writes=[Ok])
                else:
                    kb.op("act", lambda e: e.copy(out=O[:, b, c4 * 512:(c4 + 1) * 512], in_=p[:]), reads=[pk], writes=[Ok])
                n += 1
        kb.dma("pool", [(ov[:, 4 * ti:4 * ti + 4, :], O[:])], reads=[Ok], writes=[outk], st=Ok)
    kb.barrier()
    pool.release()


def emit_rmsnorm_tile(G, X, Xk, hT, hTk, gcol, sq, rt, rstd, psi=6, T=TT):
    kb = G.kb
    p, pk = G.ps[psi]
    for c in range(NCH):
        s, sk = sq[c % 2]
        kb.op("act", lambda e: e.activation(out=s[:, :T], in_=X[:, c, :], func=AF.Square), reads=[Xk], writes=[sk])
        kb.mm([(p[:, :T], G.ones_b, s[:, :T], c == 0, c == NCH - 1)], reads=[sk, G.cbk], writes=[pk])
    kb.op("act", lambda e: e.activation(out=rt[0][:, :T], in_=p[:, :T], func=AF.Sqrt, scale=1.0 / D, bias=G.eps),
          reads=[pk, G.cstk], writes=[rt[1]])
    kb.op("dve", lambda e: e.reciprocal(out=rstd[0][:, :T], in_=rt[0][:, :T]), reads=[rt[1]], writes=[rstd[1]])
    for c in range(NCH):
        kb.op("dve", lambda e: e.scalar_tensor_tensor(out=hT[:, c, :], in0=X[:, c, :], scalar=G.par[:, gcol + c:gcol + c + 1],
                                                     in1=rstd[0][:, :T], op0=ALU.mult, op1=ALU.mult),
              reads=[Xk, rstd[1], G.park], writes=[hTk])


def emit_ffn(G, src, srck, dst, dstk, gcol, w_gu, w_down, S):
    kb, pool = G.kb, G.pool
    pool.mark()
    xt = [pool.sb("xt", [128, NCH, TT], F32) for _ in range(2)]
    hT, hTk = pool.sb("hT", [128, NCH, TT], BF16)
    actT, actTk = pool.sb("actT", [128, NJ, TT], BF16)
    wgu = [pool.sb("wgu", [128, NCH, 256], BF16) for _ in range(3)]
    wdn = [pool.sb("wdn", [128, NJ, 128], BF16) for _ in range(2)]
    sq = [pool.sb("sq", [128, TT], BF16) for _ in range(2)]
    rstd = pool.sb("rstd", [128, TT], F32)
    rt = pool.sb("rt", [128, TT], F32)
    sil = [pool.sb("sil", [128, TT], F32) for _ in range(2)]
    srcv = src.rearrange("(c p) s -> p c s", p=128)
    dstv = dst.rearrange("(c p) s -> p c s", p=128)
    wguv = w_gu.rearrange("(c p) f -> p c f", p=128)
    wdnv = w_down.rearrange("(j p) d -> p j d", p=128)
    nt = S // TT

    def load(ti):
        t, k = xt[ti % 2]
        kb.dma("sp", [(t[:], srcv[:, :, ti * TT:(ti + 1) * TT])], reads=[srck[ti]], writes=[k], st=k)

    load(0)
    wi = 0
    di = 0
    for ti in range(nt):
        if ti + 1 < nt:
            load(ti + 1)
        X, Xk = xt[ti % 2]
        emit_rmsnorm_tile(G, X, Xk, hT, hTk, gcol, sq, rt, rstd)
        for j in range(NJ):
            W, Wk = wgu[wi % 3]
            wi += 1
            kb.dma("pool", [(W[:, :, 0:128], wguv[:, :, j * 128:(j + 1) * 128]),
                            (W[:, :, 128:256], wguv[:, :, FF + j * 128:FF + (j + 1) * 128])],
                   reads=[], writes=[Wk], st=Wk)
            pg, pgk = G.ps[(2 * j) % 6]
            pu, puk = G.ps[(2 * j + 1) % 6]
            kb.mm([(pg[:], W[:, c, 0:128], hT[:, c, :], c == 0, c == NCH - 1) for c in range(NCH)],
                  reads=[Wk, hTk], writes=[pgk])
            kb.mm([(pu[:], W[:, c, 128:256], hT[:, c, :], c == 0, c == NCH - 1) for c in range(NCH)],
                  reads=[Wk, hTk], writes=[puk])
            s_, s_k = sil[j % 2]
            kb.op("act", lambda e: e.activation(out=s_[:], in_=pg[:], func=AF.Silu), reads=[pgk], writes=[s_k])
            kb.op("dve", lambda e: e.tensor_tensor(out=actT[:, j, :], in0=s_[:], in1=pu[:], op=ALU.mult),
                  reads=[s_k, puk], writes=[actTk])
        for m in range(NCH):
            Wd, Wdk = wdn[di % 2]
            di += 1
            kb.dma("pool", [(Wd[:], wdnv[:, :, m * 128:(m + 1) * 128])], reads=[], writes=[Wdk], st=Wdk)
            pd, pdk = G.ps[6 + m % 2]
            kb.mm([(pd[:], Wd[:, j, :], actT[:, j, :], j == 0, j == NJ - 1) for j in range(NJ)],
                  reads=[Wdk, actTk], writes=[pdk])
            kb.op("dve", lambda e: e.scalar_tensor_tensor(out=X[:, m, :], in0=pd[:], scalar=0.5, in1=X[:, m, :],
                                                         op0=ALU.mult, op1=ALU.add),
                  reads=[pdk, Xk], writes=[Xk])
        kb.dma("sp", [(dstv[:, :, ti * TT:(ti + 1) * TT], X[:])], reads=[Xk], writes=[dstk[ti]], st=Xk)
    kb.barrier()
    pool.release()


MH, MDK, MDV = 8, 128, 256
M_INCOLS = 6160
MP_BG = 0
MP_HG = 16
MP_N = 16 + 2048


def make_mlstm_params(b_gate, head_gain):
    p = np.zeros((128, MP_N), np.float32)
    p[:, 0:8] = b_gate[0][None, :]
    p[:, 8:16] = b_gate[1][None, :]
    p[:, MP_HG:] = head_gain.reshape(1, -1)
    return p


def emit_mlstm(G, src, srck, dst, dstk, gcol, w_in, w_out, mpar_ap, S):
    kb, pool = G.kb, G.pool
    pool.mark()
    xt, xtk = pool.sb("mxt", [128, NCH, TT], F32)
    hT, hTk = pool.sb("mhT", [128, NCH, TT], BF16)
    wb = [pool.sb("mw", [128, NCH, 512], BF16) for _ in range(2)]
    wg, wgk = pool.sb("mwg", [128, NCH, 16], BF16)
    qT, qTk = pool.sb("mqT", [128, MH, TT], BF16)
    kT, kTk = pool.sb("mkT", [128, MH, TT], BF16)
    vaug, vk = pool.sb("mv", [128, 4, MH, 258], BF16)
    so, sok = pool.sb("mso", [128, 4, D], BF16)
    sig = [pool.sb("msig", [128, 512], F32) for _ in range(2)]
    mp, mpk = pool.sb("mpar", [128, MP_N], F32)
    yb = [pool.sb("my", [128, D], BF16) for _ in range(2)]
    kw = [pool.sb("mkw", [128, MH, 128], BF16) for _ in range(2)]
    sT = [pool.sb("msT", [128, MH, 128], BF16) for _ in range(2)]
    am = [pool.sb("mam", [128, MH, 128], F32) for _ in range(2)]
    CT32, CTk = pool.sb("mC32", [128, MH, 258], F32)
    CTb, CTbk = pool.sb("mCb", [128, MH, 258], BF16)
    hn = [pool.sb("mhn", [128, 256], F32) for _ in range(2)]
    junk, junkk = pool.sb("mjunk", [128, 256], F32)
    gza, gzak = pool.sb("mgz", [128, 4, 16], F32)
    gz = [pool.sb("mpbs", [128, 16], F32) for _ in range(2)]
    t1 = [pool.sb("mt1", [128, 8], F32) for _ in range(2)]
    t2 = [pool.sb("mt2", [128, 8], F32) for _ in range(2)]
    av = [pool.sb("mav", [128, 8], F32) for _ in range(2)]
    eb = [pool.sb("meb", [128, 8], F32) for _ in range(2)]
    eg = [pool.sb("meg", [128, 8], F32) for _ in range(2)]
    sm = [pool.sb("msm", [128, 8], F32) for _ in range(4)]
    xres = [pool.sb("mxr", [128, 4, TT], F32) for _ in range(2)]
    sq = [pool.sb("msq", [128, TT], BF16) for _ in range(2)]
    rstd = pool.sb("mrstd", [128, TT], F32)
    rt = pool.sb("mrt", [128, TT], F32)

    srcv = src.rearrange("(c p) s -> p c s", p=128)
    dstv = dst.rearrange("(c p) s -> p c s", p=128)
    winv = w_in.rearrange("(c p) f -> p c f", p=128)
    woutv = w_out.rearrange("(c p) f -> p c f", p=128)
    nt = S // TT

    kb.dma("sp", [(mp[:], mpar_ap)], reads=[], writes=[mpk], st=mpk)
    kb.op("pool", lambda e: e.memset(CT32[:], 0.0), writes=[CTk])
    kb.op("pool", lambda e: e.memset(CTb[:], 0.0), writes=[CTbk])
    kb.op("pool", lambda e: e.memset(vaug[:], 1.0), writes=[vk])
    wcount = [0]

    def wload(view, c0, ncols):
        W, Wk = wb[wcount[0] % 2]
        wcount[0] += 1
        kb.dma("pool", [(W[:, :, 0:ncols], view[:, :, c0:c0 + ncols])], reads=[], writes=[Wk], st=Wk)
        return W, Wk

    ecount = [0]

    def evac(out_ap, in_ap, reads, writes, scale=None):
        ecount[0] += 1
        if ecount[0] % 2 == 0:
            if scale is None:
                kb.op("dve", lambda e: e.tensor_copy(out=out_ap, in_=in_ap), reads=reads, writes=writes)
            else:
                kb.op("dve", lambda e: e.tensor_scalar(out=out_ap, in0=in_ap, scalar1=scale, scalar2=None, op0=ALU.mult),
                      reads=reads, writes=writes)
        else:
            if scale is None:
                kb.op("act", lambda e: e.copy(out=out_ap, in_=in_ap), reads=reads, writes=writes)
            else:
                kb.op("act", lambda e: e.mul(out=out_ap, in_=in_ap, mul=scale), reads=reads, writes=writes)

    for ti in range(nt):
        tsl = slice(ti * TT, (ti + 1) * TT)
        kb.dma("sp", [(xt[:], srcv[:, :, tsl])], reads=[srck[ti]], writes=[xtk], st=xtk)
        emit_rmsnorm_tile(G, xt, xtk, hT, hTk, gcol, sq, rt, rstd)
        for half in range(2):
            W, Wk = wload(winv, half * 512, 512)
            for hh in range(4):
                h = half * 4 + hh
                p, pk = newps(G)
                kb.mm([(p[:], W[:, c, hh * 128:(hh + 1) * 128], hT[:, c, :], c == 0, c == NCH - 1) for c in range(NCH)],
                      reads=[Wk, hTk], writes=[pk])
                evac(qT[:, h, :], p[:], [pk], [qTk])
        for half in range(2):
            W, Wk = wload(winv, 1024 + half * 512, 512)
            for hh in range(4):
                h = half * 4 + hh
                p, pk = newps(G)
                kb.mm([(p[:], W[:, c, hh * 128:(hh + 1) * 128], hT[:, c, :], c == 0, c == NCH - 1) for c in range(NCH)],
                      reads=[Wk, hTk], writes=[pk])
                evac(kT[:, h, :], p[:], [pk], [kTk], scale=MDK ** -0.5)
        for g in range(4):
            W, Wk = wload(winv, 2048 + g * 512, 512)
            for b in range(4):
                p, pk = newps(G)
                kb.mm([(p[:], hT[:, c, b * 128:(b + 1) * 128], W[:, c, :], c == 0, c == NCH - 1) for c in range(NCH)],
                      reads=[Wk, hTk], writes=[pk])
                evac(vaug[:, b, 2 * g:2 * g + 2, 0:256], p[:].rearrange("p (h v) -> p h v", h=2), [pk], [vk])
        for g in range(4):
            W, Wk = wload(winv, 4096 + g * 512, 512)
            for b in range(4):
                p, pk = newps(G)
                kb.mm([(p[:], hT[:, c, b * 128:(b + 1) * 128], W[:, c, :], c == 0, c == NCH - 1) for c in range(NCH)],
                      reads=[Wk, hTk], writes=[pk])
                s_, s_k = sig[(g * 4 + b) % 2]
                kb.op("act", lambda e: e.activation(out=s_[:], in_=p[:], func=AF.Sigmoid), reads=[pk], writes=[s_k])
                kb.op("pool", lambda e: e.tensor_tensor(out=so[:, b, g * 512:(g + 1) * 512], in0=s_[:],
                                                        in1=mp[:, MP_HG + g * 512:MP_HG + (g + 1) * 512], op=ALU.mult),
                      reads=[s_k, mpk], writes=[sok])
        kb.dma("pool", [(wg[:], winv[:, :, 6144:6160])], reads=[], writes=[wgk], st=wgk)
        for b in range(4):
            p, pk = newps(G)
            kb.mm([(p[:, 0:16], hT[:, c, b * 128:(b + 1) * 128], wg[:, c, :], c == 0, c == NCH - 1) for c in range(NCH)],
                  reads=[wgk, hTk], writes=[pk])
            kb.op("dve", lambda e: e.tensor_tensor(out=gza[:, b, :], in0=p[:, 0:16], in1=mp[:, 0:16], op=ALU.add),
                  reads=[pk, mpk], writes=[gzak])
        for b in range(4):
            bs = slice(b * 128, (b + 1) * 128)
            i2 = b % 2
            gz_ = gza[:, b, :]
            gzk = gzak
            t1_, t1k = t1[i2]
            t2_, t2k = t2[i2]
            kb.op("act", lambda e: e.activation(out=t1_[:], in_=gz_[:, 8:16], func=AF.Exp, scale=-1.0), reads=[gzk], writes=[t1k])
            kb.op("act", lambda e: e.activation(out=t2_[:], in_=t1_[:], func=AF.Ln, bias=G.one, scale=1.0),
                  reads=[t1k, G.cstk], writes=[t2k])
            pb, pbk = newps(G)
            kb.mm([(pb[:, 0:8], G.tri, t2_[:], True, True)], reads=[t2k, G.cstk], writes=[pbk])
            kb.mm([(pb[:, 8:16], G.ones_f, t2_[:], True, True)], reads=[t2k, G.cstk], writes=[pbk])
            a_, ak = av[i2]
            eb_, ebk = eb[i2]
            eg_, egk = eg[i2]
            pbs, pbsk = gz[i2]
            kb.op("act", lambda e: e.copy(out=pbs[:], in_=pb[:, 0:16]), reads=[pbk], writes=[pbsk])
            kb.op("dve", lambda e: e.tensor_tensor(out=a_[:], in0=pbs[:, 0:8], in1=gz_[:, 0:8], op=ALU.add),
                  reads=[pbsk, gzk], writes=[ak])
            kb.op("act", lambda e: e.activation(out=a_[:], in_=a_[:], func=AF.Exp), reads=[ak], writes=[ak])
            kb.op("act", lambda e: e.activation(out=eb_[:], in_=pbs[:, 0:8], func=AF.Exp), reads=[pbsk], writes=[ebk])
            kb.op("act", lambda e: e.activation(out=eg_[:], in_=pbs[:, 8:16], func=AF.Exp, scale=-1.0), reads=[pbsk], writes=[egk])
            am_, amk = am[i2]
            kb.op("pool", lambda e: e.tensor_tensor(out=am_[:], in0=G.tri.unsqueeze(1).broadcast_to([128, MH, 128]),
                                                    in1=a_[:].unsqueeze(2).broadcast_to([128, MH, 128]), op=ALU.mult),
                  reads=[ak, G.cstk], writes=[amk])
            pt, ptk = newps(G)
            ptb = pt[:].bitcast(BF16)
            kb.mm([(ptb[:, h * 128:(h + 1) * 128], kT[:, h, bs], G.ident_b) for h in range(MH)],
                  reads=[kTk, G.cbk], writes=[ptk], transpose=True)
            kw_, kwk = kw[i2]
            kb.op("dve", lambda e: e.tensor_tensor(out=kw_[:], in0=ptb.rearrange("p (h d) -> p h d", h=MH),
                                                   in1=a_[:].unsqueeze(2).broadcast_to([128, MH, 128]), op=ALU.mult),
                  reads=[ptk, ak], writes=[kwk])
            sT_, sTk = sT[i2]
            for hb in range(2):
                p, pk = newps(G)
                kb.mm([(p[:, hh * 128:(hh + 1) * 128], kT[:, hb * 4 + hh, bs], qT[:, hb * 4 + hh, bs], True, True) for hh in range(4)],
                      reads=[kTk, qTk], writes=[pk])
                kb.op("dve", lambda e: e.tensor_tensor(out=sT_[:, hb * 4:hb * 4 + 4, :], in0=p[:].rearrange("p (h t) -> p h t", h=4),
                                                       in1=am_[:, hb * 4:hb * 4 + 4, :], op=ALU.mult),
                      reads=[pk, amk], writes=[sTk])
            Y, Yk = yb[i2]
            for h in range(MH):
                p, pk = newps(G)
                kb.mm([(p[:, 0:257], sT_[:, h, :], vaug[:, b, h, 0:257], True, False),
                       (p[:, 0:257], qT[:, h, bs], CTb[:, h, 0:257], False, True)],
                      reads=[sTk, vk, qTk, CTbk], writes=[pk])
                s4, s4k = sm[h % 4]
                kb.op("act", lambda e: e.activation(out=s4[:, 5:6], in_=p[:, 256:257], func=AF.Abs), reads=[pk], writes=[s4k])
                kb.op("dve", lambda e: e.tensor_scalar(out=s4[:, 0:1], in0=s4[:, 5:6], scalar1=eb_[:, h:h + 1], scalar2=None,
                                                       op0=ALU.max), reads=[s4k, ebk], writes=[s4k])
                kb.op("dve", lambda e: e.reciprocal(out=s4[:, 1:2], in_=s4[:, 0:1]), reads=[s4k], writes=[s4k])
                hn_, hnk = hn[h % 2]
                kb.op("act", lambda e: e.activation(out=hn_[:], in_=p[:, 0:256], func=AF.Copy, scale=s4[:, 1:2]),
                      reads=[pk, s4k], writes=[hnk])
                kb.op("act", lambda e: e.activation(out=junk[:], in_=hn_[:], func=AF.Square, accum_out=s4[:, 2:3]),
                      reads=[hnk], writes=[junkk, s4k])
                kb.op("act", lambda e: e.activation(out=s4[:, 3:4], in_=s4[:, 2:3], func=AF.Sqrt, scale=1.0 / MDV, bias=G.eps),
                      reads=[s4k, G.cstk], writes=[s4k])
                kb.op("dve", lambda e: e.reciprocal(out=s4[:, 4:5], in_=s4[:, 3:4]), reads=[s4k], writes=[s4k])
                kb.op("dve", lambda e: e.scalar_tensor_tensor(out=Y[:, h * 256:(h + 1) * 256], in0=hn_[:], scalar=s4[:, 4:5],
                                                             in1=so[:, b, h * 256:(h + 1) * 256], op0=ALU.mult, op1=ALU.mult),
                      reads=[hnk, s4k, sok], writes=[Yk])
                pc, pck = newps(G)
                kb.mm([(pc[:, 0:257], kw_[:, h, :], vaug[:, b, h, 0:257], True, True)], reads=[kwk, vk], writes=[pck])
                kb.op("pool", lambda e: e.tensor_scalar(out=CT32[:, h, :], in0=CT32[:, h, :], scalar1=eg_[:, h:h + 1], scalar2=None,
                                                        op0=ALU.mult), reads=[egk, CTk], writes=[CTk])
                kb.op("dve", lambda e: e.scalar_tensor_tensor(out=CT32[:, h, 0:257], in0=pc[:, 0:257], scalar=eg_[:, h:h + 1],
                                                             in1=CT32[:, h, 0:257], op0=ALU.mult, op1=ALU.add),
                      reads=[pck, egk, CTk], writes=[CTk])
                kb.op("act", lambda e: e.copy(out=CTb[:, h, :], in_=CT32[:, h, :]), reads=[CTk], writes=[CTbk])
            for half in range(2):
                pt, ptk = newps(G)
                ptb = pt[:].bitcast(BF16)
                kb.mm([(ptb[:, q * 128:(q + 1) * 128], Y[:, (half * 8 + q) * 128:(half * 8 + q + 1) * 128], G.ident_b) for q in range(8)],
                      reads=[Yk, G.cbk], writes=[ptk], transpose=True)
                evac(hT[:, half * 8:half * 8 + 8, bs], ptb.rearrange("p (c t) -> p c t", c=8), [ptk], [hTk])
        for mg in range(4):
            W, Wk = wload(woutv, mg * 512, 512)
            XR, XRk = xres[mg % 2]
            kb.dma("sp", [(XR[:], srcv[:, mg * 4:mg * 4 + 4, tsl])], reads=[srck[ti]], writes=[XRk], st=XRk)
            for m4 in range(4):
                p, pk = newps(G)
                kb.mm([(p[:], W[:, c, m4 * 128:(m4 + 1) * 128], hT[:, c, :], c == 0, c == NCH - 1) for c in range(NCH)],
                      reads=[Wk, hTk], writes=[pk])
                kb.op("dve", lambda e: e.tensor_tensor(out=XR[:, m4, :], in0=p[:], in1=XR[:, m4, :], op=ALU.add),
                      reads=[pk, XRk], writes=[XRk])
            kb.dma("sp", [(dstv[:, mg * 4:mg * 4 + 4, tsl], XR[:])], reads=[XRk], writes=[dstk[ti]], st=XRk, acc=(mg > 0))
    kb.barrier()
    pool.release()


AHQ, AHKV, ADH = 32, 4, 64
A_QKV = 2560
AP_QG, AP_KG, AP_INVF = 0, 1, 2
AP_SINK = 8
AP_ROT = 24
AP_BD = 152
AP_N = 280
ROPE_THETA = 500000.0
TWO_PI = 2.0 * np.pi
CW_HI = 6.28125
CW_LO = TWO_PI - 6.28125


def make_attn_params(q_gain, k_gain, sinks):
    p = np.zeros((128, AP_N), np.float32)
    idx = np.arange(128) % 64
    p[:, AP_QG] = q_gain[idx]
    p[:, AP_KG] = k_gain[idx]
    inv_freq = (ROPE_THETA ** (-np.arange(0, 16, 2, dtype=np.float32) / 16)).astype(np.float32)
    p[:, AP_INVF] = np.where(idx < 16, inv_freq[idx % 8], 0.0)
    for par in range(2):
        for g in range(4):
            for slot in range(4):
                p[par * 64:(par + 1) * 64, AP_SINK + 4 * g + slot] = sinks[8 * g + 2 * slot + par]
    for blk in range(2):
        o = blk * 64
        for d in range(8):
            p[o + d + 8, AP_ROT + o + d] = -1.0
            p[o + d, AP_ROT + o + d + 8] = 1.0
        p[o:o + 64, AP_BD + o:AP_BD + o + 64] = 1.0
    return p


def emit_attn(G, src, srck, dst, dstk, gcol, w_qkv, w_o, apar_ap, posb_ap, S):
    kb, pool = G.kb, G.pool
    pool.mark()
    xt, xtk = pool.sb("axt", [128, NCH, TT], F32)
    hT, hTk = pool.sb("ahT", [128, NCH, TT], BF16)
    wb = [pool.sb("aw", [128, NCH, 512], BF16) for _ in range(2)]
    qT, qTk = pool.sb("aqT", [128, NCH, TT], BF16)
    kT2 = [pool.sb("akT", [128, AHKV, 128 + TT], BF16) for _ in range(2)]
    wkz = [pool.sb("awk", [128, NCH, 128], BF16) for _ in range(2)]
    vh, vhk = pool.sb("av", [128, 5, AHKV, 128], BF16)
    apar, apark = pool.sb("apar", [128, AP_N], F32)
    bd, bdk = pool.sb("abd", [128, 128], BF16)
    esink, esk = pool.sb("aes", [128, 16], F32)
    posi, posik = pool.sb("aposi", [128, TT], I32)
    ang, angk = pool.sb("aang", [128, TT], F32)
    r1, r1k = pool.sb("ar1", [128, TT], F32)
    negS, negSk = pool.sb("anS", [128, TT], F32)
    negC, negCk = pool.sb("anC", [128, TT], F32)
    sq = [pool.sb("asq", [128, TT], BF16) for _ in range(2)]
    rstd = pool.sb("arstd", [128, TT], F32)
    rt = pool.sb("art", [128, TT], F32)
    qn = [pool.sb("aqn", [128, TT], F32) for _ in range(2)]
    tu = [pool.sb("atu", [128, TT], F32) for _ in range(4)]
    pT = [pool.sb("apT", [128, 2, 2, 512], BF16) for _ in range(2)]
    dd = [pool.sb("add", [128, 512], F32) for _ in range(2)]
    xres = [pool.sb("axr", [128, 4, TT], F32) for _ in range(2)]
    mpi, mpik = pool.sb("ampi", [128, 1], F32)

    srcv = src.rearrange("(c p) s -> p c s", p=128)
    dstv = dst.rearrange("(c p) s -> p c s", p=128)
    wv = w_qkv.rearrange("(c p) f -> p c f", p=128)
    wov = w_o.rearrange("(c p) f -> p c f", p=128)
    nt = S // TT
    tri_b = G.cb[:, C_TRI:C_TRI + 128]
    ntri_b = G.cb[:, C_NTRI:C_NTRI + 128]
    ones_b64 = G.cb[:, C_ONES:C_ONES + 64]

    kb.dma("sp", [(apar[:], apar_ap)], reads=[], writes=[apark], st=apark)
    kb.op("dve", lambda e: e.tensor_copy(out=bd[:], in_=apar[:, AP_BD:AP_BD + 128]), reads=[apark], writes=[bdk])
    kb.op("act", lambda e: e.activation(out=esink[:], in_=apar[:, AP_SINK:AP_SINK + 16], func=AF.Exp), reads=[apark], writes=[esk])
    kb.op("pool", lambda e: e.memset(mpi[:], -float(np.pi)), writes=[mpik])
    for t_, k_ in wkz:
        kb.op("pool", lambda e: e.memset(t_[:], 0.0), writes=[k_])
    wcount = [0]

    def wload(view, cols):
        W, Wk = wb[wcount[0] % 2]
        wcount[0] += 1
        kb.dma("pool", [(W[:, :, d0:d0 + n], view[:, :, s0:s0 + n]) for d0, s0, n in cols], reads=[], writes=[Wk], st=Wk)
        return W, Wk

    ecount = [0]

    def qk_finish(p, pk, gain_col, out_ap, outk):
        i = ecount[0] % 2
        ecount[0] += 1
        s, sk = sq[i]
        kb.op("act", lambda e: e.activation(out=s[:], in_=p[:], func=AF.Square), reads=[pk], writes=[sk])
        p2, p2k = newps(G)
        kb.mm([(p2[:], bd[:], s[:], True, True)], reads=[sk, bdk], writes=[p2k])
        kb.op("act", lambda e: e.activation(out=rt[0][:], in_=p2[:], func=AF.Sqrt, scale=1.0 / ADH, bias=G.eps),
              reads=[p2k, G.cstk], writes=[rt[1]])
        kb.op("dve", lambda e: e.reciprocal(out=rstd[0][:], in_=rt[0][:]), reads=[rt[1]], writes=[rstd[1]])
        q_, qk_ = qn[i]
        kb.op("dve", lambda e: e.scalar_tensor_tensor(out=q_[:], in0=p[:], scalar=apar[:, gain_col:gain_col + 1], in1=rstd[0][:],
                                                     op0=ALU.mult, op1=ALU.mult), reads=[pk, rstd[1], apark], writes=[qk_])
        p3, p3k = newps(G)
        kb.mm([(p3[:], apar[:, AP_ROT:AP_ROT + 128], q_[:], True, True)], reads=[qk_, apark], writes=[p3k])
        t_, tk_ = tu[2 * i]
        u_, uk_ = tu[2 * i + 1]
        kb.op("pool", lambda e: e.tensor_tensor(out=t_[:], in0=q_[:], in1=negC[:], op=ALU.mult), reads=[qk_, negCk], writes=[tk_])
        kb.op("dve", lambda e: e.tensor_tensor(out=u_[:], in0=p3[:], in1=negS[:], op=ALU.mult), reads=[p3k, negSk], writes=[uk_])
        kb.op("pool", lambda e: e.tensor_tensor(out=out_ap, in0=t_[:], in1=u_[:], op=ALU.add), reads=[tk_, uk_], writes=[outk])

    for ti in range(nt):
        tsl = slice(ti * TT, (ti + 1) * TT)
        kb.dma("sp", [(xt[:], srcv[:, :, tsl])], reads=[srck[ti]], writes=[xtk], st=xtk)
        kb.dma("sp", [(posi[:], posb_ap[:, tsl])], reads=[], writes=[posik], st=posik)
        emit_rmsnorm_tile(G, xt, xtk, hT, hTk, gcol, sq, rt, rstd)
        kb.op("dve", lambda e: e.tensor_copy(out=ang[:], in_=posi[:]), reads=[posik], writes=[angk])
        kb.op("dve", lambda e: e.tensor_scalar(out=ang[:], in0=ang[:], scalar1=apar[:, AP_INVF:AP_INVF + 1], scalar2=None, op0=ALU.mult),
              reads=[angk, apark], writes=[angk])
        for tab, tabk, shift in ((negS, negSk, 0.0), (negC, negCk, 0.25)):
            kb.op("dve", lambda e: e.tensor_scalar(out=r1[:], in0=ang[:], scalar1=1.0 / TWO_PI, scalar2=shift, op0=ALU.mult, op1=ALU.add),
                  reads=[angk], writes=[r1k])
            kb.op("dve", lambda e: e.tensor_copy(out=posi[:], in_=r1[:]), reads=[r1k], writes=[posik])
            kb.op("dve", lambda e: e.tensor_copy(out=r1[:], in_=posi[:]), reads=[posik], writes=[r1k])
            kb.op("dve", lambda e: e.scalar_tensor_tensor(out=tab[:], in0=r1[:], scalar=-CW_HI, in1=ang[:], op0=ALU.mult, op1=ALU.add),
                  reads=[r1k, angk], writes=[tabk])
            kb.op("dve", lambda e: e.scalar_tensor_tensor(out=tab[:], in0=r1[:], scalar=-CW_LO, in1=tab[:], op0=ALU.mult, op1=ALU.add),
                  reads=[r1k, tabk], writes=[tabk])
            if shift:
                kb.op("dve", lambda e: e.tensor_scalar(out=tab[:], in0=tab[:], scalar1=float(np.pi / 2), scalar2=None, op0=ALU.add),
                      reads=[tabk], writes=[tabk])
            kb.op("dve", lambda e: e.tensor_scalar(out=r1[:], in0=tab[:], scalar1=float(np.pi), scalar2=TWO_PI, op0=ALU.is_gt, op1=ALU.mult),
                  reads=[tabk], writes=[r1k])
            kb.op("dve", lambda e: e.tensor_tensor(out=tab[:], in0=tab[:], in1=r1[:], op=ALU.subtract), reads=[tabk, r1k], writes=[tabk])
            kb.op("dve", lambda e: e.tensor_scalar(out=r1[:], in0=tab[:], scalar1=-float(np.pi), scalar2=TWO_PI, op0=ALU.is_lt, op1=ALU.mult),
                  reads=[tabk], writes=[r1k])
            kb.op("dve", lambda e: e.tensor_tensor(out=tab[:], in0=tab[:], in1=r1[:], op=ALU.add), reads=[tabk, r1k], writes=[tabk])
            kb.op("act", lambda e: e.activation(out=tab[:], in_=tab[:], func=AF.Sin), reads=[tabk], writes=[tabk])
        for kg in range(AHKV):
            for half in range(2):
                Wz, Wzk = wkz[half]
                kb.dma("pool", [(Wz[:, :, half * 64:half * 64 + 64], wv[:, :, 2048 + kg * 64:2048 + kg * 64 + 64])], reads=[], writes=[Wzk], st=Wzk)
                p, pk = newps(G)
                kb.mm([(p[:], Wz[:, c, :], hT[:, c, :], c == 0, c == NCH - 1) for c in range(NCH)], reads=[Wzk, hTk], writes=[pk])
                qk_finish(p, pk, AP_KG, kT2[half][0][:, kg, 128:128 + TT], kT2[half][1])
        W, Wk = wload(wv, [(0, 2304, 256)])
        for b in range(4):
            p, pk = newps(G)
            kb.mm([(p[:, 0:256], hT[:, c, b * 128:(b + 1) * 128], W[:, c, 0:256], c == 0, c == NCH - 1) for c in range(NCH)],
                  reads=[Wk, hTk], writes=[pk])
            for dup in range(2):
                kb.op("act", lambda e: e.copy(out=vh[:, 1 + b, :, dup * 64:dup * 64 + 64], in_=p[:, 0:256].rearrange("p (g d) -> p g d", g=AHKV)),
                      reads=[pk], writes=[vhk])
        for qg in range(4):
            W, Wk = wload(wv, [(0, qg * 512, 512)])
            for c4 in range(4):
                ch = qg * 4 + c4
                p, pk = newps(G)
                kb.mm([(p[:], W[:, c, c4 * 128:(c4 + 1) * 128], hT[:, c, :], c == 0, c == NCH - 1) for c in range(NCH)],
                      reads=[Wk, hTk], writes=[pk])
                qk_finish(p, pk, AP_QG, qT[:, ch, :], qTk)
        for b in range(4):
            gb = ti * 4 + b
            kbs = [kk for kk in (0, 1) if not (kk == 0 and gb == 0)]
            for g in range(AHKV):
                P_, Pk = pT[(b * 4 + g) % 2]
                for kk in kbs:
                    ksl = slice(b * 128 + kk * 128, b * 128 + kk * 128 + 128)
                    for par in range(2):
                        ps_, psk = newps(G)
                        kb.mm([(ps_[:, sl * 128:(sl + 1) * 128], kT2[par][0][:, g, ksl], qT[:, 4 * g + sl, b * 128:(b + 1) * 128], True, True)
                               for sl in range(4)], reads=[kT2[par][1], qTk], writes=[psk])
                        kb.op("act", lambda e: e.activation(out=P_[:, kk, par, :], in_=ps_[:], func=AF.Exp, scale=ADH ** -0.5),
                              reads=[psk], writes=[Pk])
                        msk = tri_b if kk == 1 else ntri_b
                        kb.op("dve", lambda e: e.tensor_tensor(out=P_[:, kk, par, :].rearrange("p (h q) -> p h q", h=4),
                                                               in0=P_[:, kk, par, :].rearrange("p (h q) -> p h q", h=4),
                                                               in1=msk.unsqueeze(1).broadcast_to([128, 4, 128]), op=ALU.mult),
                              reads=[Pk, G.cbk], writes=[Pk])
                d_, dk_ = dd[g % 2]
                for par in range(2):
                    prt = slice(par * 64, par * 64 + 64)
                    po, pok = newps(G)
                    kb.mm([(po[:], vh[:, b + kk, g, :], P_[:, kk, par, :], kk == kbs[0], kk == kbs[-1]) for kk in kbs],
                          reads=[vhk, Pk], writes=[pok])
                    pd, pdk = newps(G)
                    kb.mm([(pd[:], G.ones_b, P_[:, kk, par, :], kk == kbs[0], kk == kbs[-1]) for kk in kbs],
                          reads=[Pk, G.cbk], writes=[pdk])
                    kb.op("dve", lambda e: e.tensor_tensor(out=d_[prt, :].rearrange("p (h q) -> p h q", h=4),
                                                           in0=pd[prt, :].rearrange("p (h q) -> p h q", h=4),
                                                           in1=esink[prt, 4 * g:4 * g + 4].unsqueeze(2).broadcast_to([64, 4, 128]), op=ALU.add),
                          reads=[pdk, esk], writes=[dk_])
                    kb.op("dve", lambda e: e.reciprocal(out=d_[prt, :], in_=d_[prt, :]), reads=[dk_], writes=[dk_])
                    kb.op("dve", lambda e: e.tensor_tensor(out=hT[prt, 4 * g:4 * g + 4, b * 128:(b + 1) * 128],
                                                           in0=po[prt, :].rearrange("p (h q) -> p h q", h=4),
                                                           in1=d_[prt, :].rearrange("p (h q) -> p h q", h=4), op=ALU.mult),
                          reads=[pok, dk_], writes=[hTk])
        if ti + 1 < nt:
            for kt_, ktk_ in kT2:
                kb.op("pool", lambda e: e.tensor_copy(out=kt_[:, :, 0:128], in_=kt_[:, :, TT:TT + 128]), reads=[ktk_], writes=[ktk_])
            kb.op("pool", lambda e: e.tensor_copy(out=vh[:, 0, :, :], in_=vh[:, 4, :, :]), reads=[vhk], writes=[vhk])
        for mg in range(4):
            W, Wk = wload(wov, [(0, mg * 512, 512)])
            XR, XRk = xres[mg % 2]
            kb.dma("sp", [(XR[:], srcv[:, mg * 4:mg * 4 + 4, tsl])], reads=[srck[ti]], writes=[XRk], st=XRk)
            for m4 in range(4):
                p, pk = newps(G)
                kb.mm([(p[:], W[:, c, m4 * 128:(m4 + 1) * 128], hT[:, c, :], c == 0, c == NCH - 1) for c in range(NCH)],
                      reads=[Wk, hTk], writes=[pk])
                kb.op("dve", lambda e: e.tensor_tensor(out=XR[:, m4, :], in0=p[:], in1=XR[:, m4, :], op=ALU.add),
                      reads=[pk, XRk], writes=[XRk])
            kb.dma("sp", [(dstv[:, mg * 4:mg * 4 + 4, tsl], XR[:])], reads=[XRk], writes=[dstk[ti]], st=XRk, acc=(mg > 0))
    kb.barrier()
    pool.release()


def test_attn(nc, G, P, Rf, ins, xA, kA, xB, kB, S):
    w_qkv = nc.dram_tensor("w_qkv", [D, A_QKV], F32, kind="ExternalInput").ap()
    w_o = nc.dram_tensor("w_o", [D, D], F32, kind="ExternalInput").ap()
    apar = nc.dram_tensor("apar", [128, AP_N], F32, kind="ExternalInput").ap()
    posb = nc.dram_tensor("posb", [128, S], I32, kind="ExternalInput").ap()
    ins.update(w_qkv=P["attn_w_qkv"][0], w_o=P["attn_w_o"][0],
               apar=make_attn_params(P["attn_q_gain"][0], P["attn_k_gain"][0], P["attn_sinks"][0]),
               posb=np.ascontiguousarray(np.broadcast_to(Rf["pos"][None, :].astype(np.int32), (128, S))))
    emit_attn(G, xA, kA, xB, kB, 0, w_qkv, w_o, apar, posb, S)


RH, RN = 32, 64
RC = 64
RP_MIX, RP_W0, RP_A0, RP_KK, RP_KA, RP_RK, RP_LNW, RP_LNB = 0, 96, 112, 128, 144, 160, 176, 192
RP_N = 208
RK_RM = 0
RK_SL = 512
RK_SU = 640
RK_IU = 768
RK_BD = 896
RK_ID = 1024
RK_N = 1152
RWKV_LN_EPS = 64e-5
EM05 = float(np.exp(-0.5))


def make_rwkv_params(mix, w0, a0, k_k, k_a, r_k, ln_w, ln_b):
    p = np.zeros((128, RP_N), np.float32)

    def lay(v):
        return np.ascontiguousarray(v.reshape(16, 128).T)

    for i in range(6):
        p[:, RP_MIX + 16 * i:RP_MIX + 16 * (i + 1)] = lay(mix[i])
    p[:, RP_W0:RP_W0 + 16] = lay(w0)
    p[:, RP_A0:RP_A0 + 16] = lay(a0)
    p[:, RP_KK:RP_KK + 16] = lay(k_k)
    p[:, RP_KA:RP_KA + 16] = lay(k_a)
    p[:, RP_RK:RP_RK + 16] = lay(r_k.reshape(-1))
    p[:, RP_LNW:RP_LNW + 16] = lay(ln_w)
    p[:, RP_LNB:RP_LNB + 16] = lay(ln_b)
    return p


def make_rwkv_consts():
    c = np.zeros((128, RK_N), np.float32)
    c[:, RK_RM:RK_RM + 512] = (np.arange(512) % RC != 0).astype(np.float32)[None, :]
    pi = np.arange(128)[:, None]
    fi = np.arange(128)[None, :]
    same = (pi // 64) == (fi // 64)
    c[:, RK_SL:RK_SL + 128] = (same & (pi % 64 > fi % 64))
    c[:, RK_SU:RK_SU + 128] = (same & (pi % 64 < fi % 64))
    c[:, RK_IU:RK_IU + 128] = (same & (pi % 64 <= fi % 64))
    c[:, RK_BD:RK_BD + 128] = same
    c[:, RK_ID:RK_ID + 128] = np.eye(128)
    return c


def emit_rwkv_stage1(G, src, srck, gcol, W, rpar_ap, rk_ap, scr, scrk, S):
    kb, pool = G.kb, G.pool
    pool.mark()
    xt, xtk = pool.sb("rxt", [128, NCH, TT], F32)
    dx, dxk = pool.sb("rdx", [128, NCH, TT], BF16)
    xm = [pool.sb("rxm", [128, NCH, TT], BF16) for _ in range(2)]
    wb = [pool.sb("rw", [128, NCH, 512], BF16) for _ in range(2)]
    wla, wlak = pool.sb("rwla", [128, NCH, 96], BF16)
    ala, alak = pool.sb("rala", [128, NCH, 96], BF16)
    gla, glak = pool.sb("rgla", [128, NCH, 256], BF16)
    wlb, wlbk = pool.sb("rwlb", [96, D], BF16)
    alb, albk = pool.sb("ralb", [96, D], BF16)
    glb, glbk = pool.sb("rglb", [128, 2, D], BF16)
    tw, twk = pool.sb("rtw", [96, TT], BF16)
    ta, tak = pool.sb("rta", [96, TT], BF16)
    tg, tgk = pool.sb("rtg", [128, 2, TT], BF16)
    rp, rpk = pool.sb("rpar", [128, RP_N], F32)
    rc_, rck = pool.sb("rcst", [128, RK_N], F32)
    bdb, bdbk = pool.sb("rbdb", [128, 128], BF16)
    omka, omkak = pool.sb("romka", [128, 16], F32)
    carry, carryk = pool.sb("rcarry", [128, NCH, 1], F32)
    sq = [pool.sb("rsq", [128, TT], BF16) for _ in range(2)]
    rstd = pool.sb("rrstd", [128, TT], F32)
    rt = pool.sb("rrt", [128, TT], F32)
    NTMP = 16
    tmp = [pool.sb("rtmp", [128, TT], F32) for _ in range(NTMP)]
    gcs, gcsk = pool.sb("rgcs", [128, NCH, TT // RC], F32)

    srcv = src.rearrange("(c p) s -> p c s", p=128)
    nt = S // TT
    Wv = {k: (v.rearrange("(c p) f -> p c f", p=128) if k in ("wr", "wk", "wv", "wla", "ala", "gla") else v) for k, v in W.items()}
    scv = {k: v.rearrange("(c p) s -> p c s", p=128) for k, v in scr.items()}

    kb.dma("sp", [(rp[:], rpar_ap)], reads=[], writes=[rpk], st=rpk)
    kb.dma("sp", [(rc_[:], rk_ap)], reads=[], writes=[rck], st=rck)
    kb.dma("pool", [(wla[:], Wv["wla"])], reads=[], writes=[wlak], st=wlak)
    kb.dma("pool", [(ala[:], Wv["ala"])], reads=[], writes=[alak], st=alak)
    kb.dma("pool", [(gla[:], Wv["gla"])], reads=[], writes=[glak], st=glak)
    kb.dma("pool", [(wlb[:], W["wlb"])], reads=[], writes=[wlbk], st=wlbk)
    kb.dma("pool", [(alb[:], W["alb"])], reads=[], writes=[albk], st=albk)
    kb.dma("pool", [(glb[:], W["glb"].rearrange("(j p) f -> p j f", p=128))], reads=[], writes=[glbk], st=glbk)
    kb.op("dve", lambda e: e.tensor_copy(out=bdb[:], in_=rc_[:, RK_BD:RK_BD + 128]), reads=[rck], writes=[bdbk])
    kb.op("dve", lambda e: e.tensor_scalar(out=omka[:], in0=rp[:, RP_KA:RP_KA + 16], scalar1=-1.0, scalar2=1.0, op0=ALU.mult, op1=ALU.add),
          reads=[rpk], writes=[omkak])
    kb.op("pool", lambda e: e.memset(carry[:], 0.0), writes=[carryk])
    wcount = [0]
    tcount = [0]

    def wload(view, c0):
        Wt, Wk = wb[wcount[0] % 2]
        wcount[0] += 1
        kb.dma("pool", [(Wt[:], view[:, :, c0:c0 + 512])], reads=[], writes=[Wk], st=Wk)
        return Wt, Wk

    def T_():
        tcount[0] += 1
        return tmp[tcount[0] % NTMP]

    def mixin(i, buf):
        X, Xk = xm[buf]
        for c in range(NCH):
            eng = "dve"
            if eng == "dve":
                kb.op("dve", lambda e: e.scalar_tensor_tensor(out=X[:, c, :], in0=dx[:, c, :], scalar=rp[:, RP_MIX + 16 * i + c:RP_MIX + 16 * i + c + 1],
                                                             in1=xt[:, c, :], op0=ALU.mult, op1=ALU.add), reads=[dxk, xtk, rpk], writes=[Xk])
            else:
                t_, tk_ = T_()
                kb.op("pool", lambda e: e.tensor_scalar(out=t_[:], in0=dx[:, c, :], scalar1=rp[:, RP_MIX + 16 * i + c:RP_MIX + 16 * i + c + 1],
                                                        scalar2=None, op0=ALU.mult), reads=[dxk, rpk], writes=[tk_])
                kb.op("pool", lambda e: e.tensor_tensor(out=X[:, c, :], in0=t_[:], in1=xt[:, c, :], op=ALU.add), reads=[tk_, xtk], writes=[Xk])
        return X, Xk

    def store(name, c, tsl, t_, tk_, ti):
        kb.dma("sp", [(scv[name][:, c, tsl], t_[:])], reads=[tk_], writes=[scrk[name][ti]], st=tk_, acc=(c > 0))

    for ti in range(nt):
        tsl = slice(ti * TT, (ti + 1) * TT)
        kb.dma("sp", [(xt[:], srcv[:, :, tsl])], reads=[srck[ti]], writes=[xtk], st=xtk)
        p, pk = newps(G)
        for c in range(NCH):
            s, sk = sq[c % 2]
            kb.op("act", lambda e: e.activation(out=s[:], in_=xt[:, c, :], func=AF.Square), reads=[xtk], writes=[sk])
            kb.mm([(p[:], G.ones_b, s[:], c == 0, c == NCH - 1)], reads=[sk, G.cbk], writes=[pk])
        kb.op("act", lambda e: e.activation(out=rt[0][:], in_=p[:], func=AF.Sqrt, scale=1.0 / D, bias=G.eps), reads=[pk, G.cstk], writes=[rt[1]])
        kb.op("dve", lambda e: e.reciprocal(out=rstd[0][:], in_=rt[0][:]), reads=[rt[1]], writes=[rstd[1]])
        for c in range(NCH):
            kb.op("dve", lambda e: e.scalar_tensor_tensor(out=xt[:, c, :], in0=xt[:, c, :], scalar=G.par[:, gcol + c:gcol + c + 1], in1=rstd[0][:],
                                                         op0=ALU.mult, op1=ALU.mult), reads=[xtk, rstd[1], G.park], writes=[xtk])
        kb.op("pool", lambda e: e.tensor_tensor(out=dx[:, :, 1:TT], in0=xt[:, :, 0:TT - 1], in1=xt[:, :, 1:TT], op=ALU.subtract),
              reads=[xtk], writes=[dxk])
        kb.op("pool", lambda e: e.tensor_tensor(out=dx[:, :, 0:1], in0=carry[:], in1=xt[:, :, 0:1], op=ALU.subtract),
              reads=[xtk, carryk], writes=[dxk])
        kb.op("pool", lambda e: e.tensor_copy(out=carry[:], in_=xt[:, :, TT - 1:TT]), reads=[xtk], writes=[carryk])
        X, Xk = mixin(1, 0)
        p, pk = newps(G)
        kb.mm([(p[0:96, :], wla[:, c, :], X[:, c, :], c == 0, c == NCH - 1) for c in range(NCH)], reads=[wlak, Xk], writes=[pk])
        kb.op("act", lambda e: e.activation(out=tw[:], in_=p[0:96, :], func=AF.Tanh), reads=[pk], writes=[twk])
        X, Xk = mixin(4, 1)
        p, pk = newps(G)
        kb.mm([(p[0:96, :], ala[:, c, :], X[:, c, :], c == 0, c == NCH - 1) for c in range(NCH)], reads=[alak, Xk], writes=[pk])
        kb.op("act", lambda e: e.copy(out=ta[:], in_=p[0:96, :]), reads=[pk], writes=[tak])
        X, Xk = mixin(5, 0)
        for j in range(2):
            p, pk = newps(G)
            kb.mm([(p[:], gla[:, c, j * 128:(j + 1) * 128], X[:, c, :], c == 0, c == NCH - 1) for c in range(NCH)], reads=[glak, Xk], writes=[pk])
            kb.op("act", lambda e: e.activation(out=tg[:, j, :], in_=p[:], func=AF.Sigmoid), reads=[pk], writes=[tgk])
        for c in range(NCH):
            p, pk = newps(G)
            kb.mm([(p[:], glb[:, j, c * 128:(c + 1) * 128], tg[:, j, :], j == 0, j == 1) for j in range(2)], reads=[glbk, tgk], writes=[pk])
            t_, tk_ = T_()
            kb.op("act", lambda e: e.copy(out=t_[:], in_=p[:]), reads=[pk], writes=[tk_])
            store("g", c, tsl, t_, tk_, ti)
        Xr, Xrk = mixin(0, 1)
        Xk_, Xkk = mixin(2, 0)
        per_c = {}
        for c4 in range(4):
            Wr, Wrk = wload(Wv["wr"], c4 * 512)
            Wk2, Wk2k = wload(Wv["wk"], c4 * 512)
            for cc in range(4):
                c = c4 * 4 + cc
                col = slice(cc * 128, (cc + 1) * 128)
                pr, prk = newps(G)
                kb.mm([(pr[:], Wr[:, k, col], Xr[:, k, :], k == 0, k == NCH - 1) for k in range(NCH)], reads=[Wrk, Xrk], writes=[prk])
                pkk, pkkk = newps(G)
                kb.mm([(pkk[:], Wk2[:, k, col], Xk_[:, k, :], k == 0, k == NCH - 1) for k in range(NCH)], reads=[Wk2k, Xkk], writes=[pkkk])
                pa, pak = newps(G)
                kb.mm([(pa[:], alb[:, c * 128:(c + 1) * 128], ta[:], True, True)], reads=[albk, tak], writes=[pak])
                pw, pwk = newps(G)
                kb.mm([(pw[:], wlb[:, c * 128:(c + 1) * 128], tw[:], True, True)], reads=[wlbk, twk], writes=[pwk])
                cs = lambda b, c=c: rp[:, b + c:b + c + 1]
                a_, ak = T_()
                kb.op("act", lambda e: e.activation(out=a_[:], in_=pa[:], func=AF.Sigmoid, bias=cs(RP_A0), scale=1.0), reads=[pak, rpk], writes=[ak])
                lw, lwk = T_()
                kb.op("act", lambda e: e.activation(out=lw[:], in_=pw[:], func=AF.Sigmoid, bias=cs(RP_W0), scale=1.0), reads=[pwk, rpk], writes=[lwk])
                kb.op("act", lambda e: e.mul(out=lw[:], in_=lw[:], mul=-EM05), reads=[lwk], writes=[lwk])
                L, Lk = T_()
                kb.op("dve", lambda e: e.tensor_tensor_scan(out=L[:], data0=rc_[:, RK_RM:RK_RM + TT], data1=lw[:], initial=0.0,
                                                            op0=ALU.mult, op1=ALU.add), reads=[lwk, rck], writes=[Lk])
                Gm, Gmk = T_()
                Gi, Gik = T_()
                Gp, Gpk = T_()
                kb.op("act", lambda e: e.activation(out=Gm[:], in_=L[:], func=AF.Exp), reads=[Lk], writes=[Gmk])
                kb.op("act", lambda e: e.activation(out=Gi[:], in_=L[:], func=AF.Exp, scale=-1.0), reads=[Lk], writes=[Gik])
                kb.op("dve", lambda e: e.tensor_tensor(out=Gp[:], in0=L[:], in1=lw[:], op=ALU.subtract), reads=[Lk, lwk], writes=[Gpk])
                kb.op("act", lambda e: e.activation(out=Gp[:], in_=Gp[:], func=AF.Exp), reads=[Gpk], writes=[Gpk])
                kb.op("pool", lambda e: e.tensor_copy(out=gcs[:, c, :], in_=Gm[:].rearrange("p (j t) -> p j t", t=RC)[:, :, RC - 1]),
                      reads=[Gmk], writes=[gcsk])
                kkr, kkrk = T_()
                kb.op("dve", lambda e: e.tensor_scalar(out=kkr[:], in0=pkk[:], scalar1=cs(RP_KK), scalar2=None, op0=ALU.mult), reads=[pkkk, rpk], writes=[kkrk])
                s, sk = sq[c % 2]
                kb.op("act", lambda e: e.activation(out=s[:], in_=kkr[:], func=AF.Square), reads=[kkrk], writes=[sk])
                pn, pnk = newps(G)
                kb.mm([(pn[:], bdb[:], s[:], True, True)], reads=[sk, bdbk], writes=[pnk])
                nr, nrk = T_()
                kb.op("act", lambda e: e.activation(out=nr[:], in_=pn[:], func=AF.Sqrt), reads=[pnk], writes=[nrk])
                kb.op("dve", lambda e: e.tensor_scalar(out=nr[:], in0=nr[:], scalar1=1e-12, scalar2=None, op0=ALU.max), reads=[nrk], writes=[nrk])
                kb.op("dve", lambda e: e.reciprocal(out=nr[:], in_=nr[:]), reads=[nrk], writes=[nrk])
                kb.op("dve", lambda e: e.tensor_tensor(out=kkr[:], in0=kkr[:], in1=nr[:], op=ALU.mult), reads=[kkrk, nrk], writes=[kkrk])
                km, kmk = T_()
                kb.op("pool", lambda e: e.tensor_scalar(out=km[:], in0=a_[:], scalar1=cs(RP_KA), scalar2=omka[:, c:c + 1], op0=ALU.mult, op1=ALU.add),
                      reads=[ak, rpk, omkak], writes=[kmk])
                kb.op("dve", lambda e: e.tensor_tensor(out=km[:], in0=pkk[:], in1=km[:], op=ALU.mult), reads=[pkkk, kmk], writes=[kmk])
                rr, rrk = T_()
                kb.op("act", lambda e: e.copy(out=rr[:], in_=pr[:]), reads=[prk], writes=[rrk])
                s2, s2k = sq[(c + 1) % 2]
                kb.op("dve", lambda e: e.scalar_tensor_tensor(out=s2[:], in0=rr[:], scalar=cs(RP_RK), in1=km[:], op0=ALU.mult, op1=ALU.mult),
                      reads=[rrk, kmk, rpk], writes=[s2k])
                pb, pbk = newps(G)
                kb.mm([(pb[:], bdb[:], s2[:], True, True)], reads=[s2k, bdbk], writes=[pbk])
                bs_, bsk = T_()
                kb.op("act", lambda e: e.copy(out=bs_[:], in_=pb[:]), reads=[pbk], writes=[bsk])
                store("bs", c, tsl, bs_, bsk, ti)
                kb.op("dve", lambda e: e.tensor_tensor(out=rr[:], in0=rr[:], in1=Gm[:], op=ALU.mult), reads=[rrk, Gmk], writes=[rrk])
                store("rt", c, tsl, rr, rrk, ti)
                kb.op("dve", lambda e: e.tensor_tensor(out=km[:], in0=km[:], in1=Gi[:], op=ALU.mult), reads=[kmk, Gik], writes=[kmk])
                store("kt", c, tsl, km, kmk, ti)
                kb.op("pool", lambda e: e.tensor_tensor(out=a_[:], in0=a_[:], in1=kkr[:], op=ALU.mult), reads=[ak, kkrk], writes=[ak])
                kb.op("dve", lambda e: e.tensor_tensor(out=a_[:], in0=a_[:], in1=Gi[:], op=ALU.mult), reads=[ak, Gik], writes=[ak])
                store("bt", c, tsl, a_, ak, ti)
                kb.op("dve", lambda e: e.scalar_tensor_tensor(out=kkr[:], in0=kkr[:], scalar=-1.0, in1=Gp[:], op0=ALU.mult, op1=ALU.mult),
                      reads=[kkrk, Gpk], writes=[kkrk])
                store("at", c, tsl, kkr, kkrk, ti)
        kb.dma("sp", [(scr["gc"].rearrange("(c p) j -> p c j", p=128)[:, :, ti * (TT // RC):(ti + 1) * (TT // RC)], gcs[:])],
               reads=[gcsk], writes=[scrk["gc"][ti]], st=gcsk)
        Xv, Xvk = mixin(3, 1)
        for c4 in range(4):
            Wv_, Wvk = wload(Wv["wv"], c4 * 512)
            for cc in range(4):
                c = c4 * 4 + cc
                pv, pvk = newps(G)
                kb.mm([(pv[:], Wv_[:, k, cc * 128:(cc + 1) * 128], Xv[:, k, :], k == 0, k == NCH - 1) for k in range(NCH)],
                      reads=[Wvk, Xvk], writes=[pvk])
                t_, tk_ = T_()
                kb.op("act", lambda e: e.copy(out=t_[:], in_=pv[:]), reads=[pvk], writes=[tk_])
                store("v", c, tsl, t_, tk_, ti)
    kb.barrier()
    pool.release()


def emit_rwkv_stage2(G, rk_ap, scr, scrk, S):
    kb, pool = G.kb, G.pool
    pool.mark()
    NG = 4
    rc_, rck = pool.sb("r2cst", [128, RK_N], F32)
    kb.dma("sp", [(rc_[:], rk_ap)], reads=[], writes=[rck], st=rck)
    ident = rc_[:, RK_ID:RK_ID + 128]
    nchunk = S // RC

    def bdtile(name, n=2):
        ts = []
        for _ in range(n):
            t, k = pool.sb(name, [128, 4, 128], F32)
            kb.op("pool", lambda e: e.memset(t[:], 0.0), writes=[k])
            ts.append((t, k))
        return ts

    A_ = bdtile("r2a")
    B_ = bdtile("r2b")
    K_ = bdtile("r2k")
    R_ = bdtile("r2r")
    V_ = bdtile("r2v")
    plain = lambda name, n=2: [pool.sb(name, [128, 4, 128], F32) for _ in range(n)]
    Bt, Kt, Vt = plain("r2bt"), plain("r2kt"), plain("r2vt")
    Pm, Qm = plain("r2P", 4), plain("r2Q", 4)
    Zm = plain("r2Z", 4)
    Mak, Mrb, Mrk = plain("r2mak"), plain("r2mrb"), plain("r2mrk")
    RHS, U = plain("r2rhs"), plain("r2u")
    Yt = [pool.sb("r2y", [128, 4, RC], F32) for _ in range(2)]
    tmpS = plain("r2ts")
    ST = []
    for g in range(NG):
        t, k = pool.sb("r2ST", [128, 4, 128], F32)
        kb.op("pool", lambda e: e.memset(t[:], 0.0), writes=[k])
        ST.append((t, k))
    gc = []
    gcv = scr["gc"].rearrange("(q p) j -> p q j", p=128)
    for g in range(NG):
        t, k = pool.sb("r2gc", [128, 4, nchunk], F32)
        kb.dma("sp", [(t[:], gcv[:, 4 * g:4 * g + 4, :])], reads=list(scrk["gc"]), writes=[k], st=k)
        gc.append((t, k))
    scv = {k: v.rearrange("(q p) s -> p q s", p=128) for k, v in scr.items() if k != "gc"}
    ev = [0]

    def mask_evac(out_t, ps, mcol):
        o, ok = out_t
        p, pk = ps
        kb.op("dve", lambda e: e.tensor_tensor(out=o[:], in0=p[:].rearrange("p (q f) -> p q f", q=4),
                                               in1=rc_[:, mcol:mcol + 128].unsqueeze(1).broadcast_to([128, 4, 128]), op=ALU.mult),
              reads=[pk, rck], writes=[ok])

    def copy_evac(out_t, ps):
        o, ok = out_t
        p, pk = ps
        ev[0] += 1
        if ev[0] % 2:
            kb.op("act", lambda e: e.copy(out=o[:], in_=p[:].rearrange("p (q f) -> p q f", q=4)), reads=[pk], writes=[ok])
        else:
            kb.op("dve", lambda e: e.tensor_copy(out=o[:], in_=p[:].rearrange("p (q f) -> p q f", q=4)), reads=[pk], writes=[ok])

    def mm4(lhs, rhs, extra=None):
        ps = newps(G)
        p, pk = ps
        terms = [(lhs, rhs)] + (extra or [])
        reads = []
        for l, r in terms:
            reads += [l[1], r[1]]
        mms = []
        for q in range(4):
            for i, (l, r) in enumerate(terms):
                mms.append((p[:, q * 128:(q + 1) * 128], l[0][:, q, :], r[0][:, q, :], i == 0, i == len(terms) - 1))
        kb.mm(mms, reads=reads, writes=[pk])
        return ps

    it = 0
    for ci in range(nchunk):
        csl = slice(ci * RC, (ci + 1) * RC)
        ti = (ci * RC) // TT
        for g in range(NG):
            i2 = it % 2
            it += 1
            for name, tl in (("at", A_), ("bt", B_), ("kt", K_), ("rt", R_), ("v", V_)):
                t, k = tl[i2]
                kb.dma("sp", [(t[hh * 64:(hh + 1) * 64, :, hh * 64:(hh + 1) * 64], scv[name][hh * 64:(hh + 1) * 64, 4 * g:4 * g + 4, csl]) for hh in range(2)],
                       reads=[scrk[name][ti]], writes=[k], st=k)
            A, B, K, R, V = A_[i2], B_[i2], K_[i2], R_[i2], V_[i2]
            for srcT, dstT in ((B, Bt[i2]), (K, Kt[i2]), (V, Vt[i2])):
                ps = newps(G)
                kb.mm([(ps[0][:, q * 128:(q + 1) * 128], srcT[0][:, q, :], ident) for q in range(4)], reads=[srcT[1], rck], writes=[ps[1]], transpose=True)
                copy_evac(dstT, ps)
            P0, Q0 = Pm[2 * i2], Qm[2 * i2]
            mask_evac(P0, mm4(A, B), RK_SL)
            mask_evac(Q0, mm4(B, A), RK_SU)
            mask_evac(Mak[i2], mm4(K, A), RK_SU)
            mask_evac(Mrb[i2], mm4(B, R), RK_IU)
            mask_evac(Mrk[i2], mm4(K, R), RK_IU)
            Z = Zm[2 * i2]
            kb.op("pool", lambda e: e.tensor_tensor(out=Z[0][:], in0=Q0[0][:], in1=ident.unsqueeze(1).broadcast_to([128, 4, 128]), op=ALU.add),
                  reads=[Q0[1], rck], writes=[Z[1]])
            Pc, Qc = P0, Q0
            for lvl in range(1, 6):
                Pn, Qn = Pm[2 * i2 + (lvl % 2)], Qm[2 * i2 + (lvl % 2)]
                psP = mm4(Qc, Pc)
                psQ = mm4(Pc, Qc) if lvl < 5 else None
                copy_evac(Pn, psP)
                if psQ is not None:
                    copy_evac(Qn, psQ)
                psZ = mm4(Pn, Z)
                Zn = Zm[2 * i2 + (lvl % 2)]
                kb.op("dve", lambda e: e.tensor_tensor(out=Zn[0][:], in0=psZ[0][:].rearrange("p (q f) -> p q f", q=4), in1=Z[0][:], op=ALU.add),
                      reads=[psZ[1], Z[1]], writes=[Zn[1]])
                Z = Zn
                Pc, Qc = Pn, Qn
            Sg = ST[g]
            copy_evac(RHS[i2], mm4(A, Sg, [(Mak[i2], Vt[i2])]))
            copy_evac(U[i2], mm4(Z, RHS[i2]))
            psY = mm4(Sg, R, [(U[i2], Mrb[i2]), (Vt[i2], Mrk[i2])])
            Y, Yk = Yt[i2]
            pY = psY[0][:].rearrange("p (q f) -> p q f", q=4)
            kb.op("act", lambda e: e.copy(out=Y[0:64, :, :], in_=pY[0:64, :, 0:64]), reads=[psY[1]], writes=[Yk])
            kb.op("act", lambda e: e.copy(out=Y[64:128, :, :], in_=pY[64:128, :, 64:128]), reads=[psY[1]], writes=[Yk])
            kb.dma("pool", [(scv["y"][:, 4 * g:4 * g + 4, csl], Y[:])], reads=[Yk], writes=[scrk["y"][ti]], st=Yk, acc=True)
            psS = mm4(Bt[i2], U[i2], [(Kt[i2], Vt[i2])])
            tS = tmpS[i2]
            kb.op("dve", lambda e: e.tensor_tensor(out=tS[0][:], in0=psS[0][:].rearrange("p (q f) -> p q f", q=4), in1=Sg[0][:], op=ALU.add),
                  reads=[psS[1], Sg[1]], writes=[tS[1]])
            kb.op("pool", lambda e: e.tensor_tensor(out=Sg[0][:], in0=tS[0][:],
                                                    in1=gc[g][0][:, :, ci:ci + 1].broadcast_to([128, 4, 128]), op=ALU.mult),
                  reads=[tS[1], gc[g][1]], writes=[Sg[1]])
    kb.barrier()
    pool.release()


def emit_rwkv_stage3(G, src, srck, dst, dstk, w_o, rpar_ap, rk_ap, scr, scrk, S):
    kb, pool = G.kb, G.pool
    pool.mark()
    rp, rpk = pool.sb("r3par", [128, RP_N], F32)
    rc_, rck = pool.sb("r3cst", [128, RK_N], F32)
    kb.dma("sp", [(rp[:], rpar_ap)], reads=[], writes=[rpk], st=rpk)
    kb.dma("sp", [(rc_[:], rk_ap)], reads=[], writes=[rck], st=rck)
    bd = rc_[:, RK_BD:RK_BD + 128]
    lneps, lnepsk = pool.sb("r3eps", [128, 1], F32)
    kb.op("pool", lambda e: e.memset(lneps[:], RWKV_LN_EPS), writes=[lnepsk])
    ins = {n: [pool.sb("r3" + n, [128, 4, TT], F32) for _ in range(2)] for n in ("y", "bs", "v", "g")}
    zT, zTk = pool.sb("r3z", [128, NCH, TT], BF16)
    wb = [pool.sb("r3w", [128, NCH, 512], BF16) for _ in range(2)]
    xres = [pool.sb("r3xr", [128, 4, TT], F32) for _ in range(2)]
    NT = 8
    tmp = [pool.sb("r3t", [128, TT], F32) for _ in range(NT)]
    tc_ = [0]

    def T_():
        tc_[0] += 1
        return tmp[tc_[0] % NT]

    srcv = src.rearrange("(c p) s -> p c s", p=128)
    dstv = dst.rearrange("(c p) s -> p c s", p=128)
    wov = w_o.rearrange("(c p) f -> p c f", p=128)
    scv = {k: v.rearrange("(c p) s -> p c s", p=128) for k, v in scr.items() if k != "gc"}
    nt = S // TT
    it = 0
    wc = 0
    for ti in range(nt):
        tsl = slice(ti * TT, (ti + 1) * TT)
        for c4 in range(4):
            i2 = it % 2
            it += 1
            for n in ("y", "bs", "v", "g"):
                t, k = ins[n][i2]
                kb.dma("sp", [(t[:], scv[n][:, 4 * c4:4 * c4 + 4, tsl])], reads=[scrk[n][ti]], writes=[k], st=k)
            Yt, Ytk = ins["y"][i2]
            Bs, Bsk = ins["bs"][i2]
            Vv, Vvk = ins["v"][i2]
            Gg, Ggk = ins["g"][i2]
            for cc in range(4):
                c = 4 * c4 + cc
                pm, pmk = newps(G)
                kb.mm([(pm[:], bd, Yt[:, cc, :], True, True)], reads=[Ytk, rck], writes=[pmk])
                s, sk = T_()
                kb.op("act", lambda e: e.activation(out=s[:], in_=Yt[:, cc, :], func=AF.Square), reads=[Ytk], writes=[sk])
                pq, pqk = newps(G)
                kb.mm([(pq[:], bd, s[:], True, True)], reads=[sk, rck], writes=[pqk])
                mu, muk = T_()
                kb.op("act", lambda e: e.mul(out=mu[:], in_=pm[:], mul=1.0 / RN), reads=[pmk], writes=[muk])
                m2, m2k = T_()
                kb.op("pool", lambda e: e.tensor_tensor(out=m2[:], in0=mu[:], in1=mu[:], op=ALU.mult), reads=[muk], writes=[m2k])
                kb.op("dve", lambda e: e.scalar_tensor_tensor(out=m2[:], in0=pq[:], scalar=1.0 / RN, in1=m2[:], op0=ALU.mult, op1=ALU.subtract),
                      reads=[pqk, m2k], writes=[m2k])
                kb.op("act", lambda e: e.activation(out=m2[:], in_=m2[:], func=AF.Sqrt, bias=lneps[:, 0:1], scale=1.0), reads=[m2k, lnepsk], writes=[m2k])
                kb.op("dve", lambda e: e.reciprocal(out=m2[:], in_=m2[:]), reads=[m2k], writes=[m2k])
                kb.op("pool", lambda e: e.tensor_tensor(out=mu[:], in0=Yt[:, cc, :], in1=mu[:], op=ALU.subtract), reads=[Ytk, muk], writes=[muk])
                kb.op("dve", lambda e: e.tensor_tensor(out=mu[:], in0=mu[:], in1=m2[:], op=ALU.mult), reads=[muk, m2k], writes=[muk])
                kb.op("dve", lambda e: e.tensor_scalar(out=mu[:], in0=mu[:], scalar1=rp[:, RP_LNW + c:RP_LNW + c + 1], scalar2=rp[:, RP_LNB + c:RP_LNB + c + 1],
                                                       op0=ALU.mult, op1=ALU.add), reads=[muk, rpk], writes=[muk])
                bn, bnk = T_()
                kb.op("pool", lambda e: e.tensor_tensor(out=bn[:], in0=Bs[:, cc, :], in1=Vv[:, cc, :], op=ALU.mult), reads=[Bsk, Vvk], writes=[bnk])
                kb.op("pool", lambda e: e.tensor_tensor(out=mu[:], in0=mu[:], in1=bn[:], op=ALU.add), reads=[muk, bnk], writes=[muk])
                kb.op("dve", lambda e: e.tensor_tensor(out=zT[:, c, :], in0=mu[:], in1=Gg[:, cc, :], op=ALU.mult), reads=[muk, Ggk], writes=[zTk])
        for mg in range(4):
            W, Wk = wb[wc % 2]
            wc += 1
            kb.dma("pool", [(W[:], wov[:, :, mg * 512:(mg + 1) * 512])], reads=[], writes=[Wk], st=Wk)
            XR, XRk = xres[mg % 2]
            kb.dma("sp", [(XR[:], srcv[:, mg * 4:mg * 4 + 4, tsl])], reads=[srck[ti]], writes=[XRk], st=XRk)
            for m4 in range(4):
                p, pk = newps(G)
                kb.mm([(p[:], W[:, c, m4 * 128:(m4 + 1) * 128], zT[:, c, :], c == 0, c == NCH - 1) for c in range(NCH)],
                      reads=[Wk, zTk], writes=[pk])
                kb.op("dve", lambda e: e.tensor_tensor(out=XR[:, m4, :], in0=p[:], in1=XR[:, m4, :], op=ALU.add),
                      reads=[pk, XRk], writes=[XRk])
            kb.dma("sp", [(dstv[:, mg * 4:mg * 4 + 4, tsl], XR[:])], reads=[XRk], writes=[dstk[ti]], st=XRk, acc=(mg > 0))
    kb.barrier()
    pool.release()


RW_SCR = ("at", "bt", "kt", "rt", "v", "bs", "g", "y")


def emit_rwkv(G, nc, src, srck, dst, dstk, gcol, W, rpar_ap, rk_ap, S, tag="r"):
    scr = {n: nc.dram_tensor(f"{tag}_{n}", [D, S], F32, kind="Internal").ap() for n in RW_SCR}
    scr["gc"] = nc.dram_tensor(f"{tag}_gc", [D, S // RC], F32, kind="Internal").ap()
    scrk = {n: [G.kb.tk(f"{tag}{n}") for _ in range(S // TT)] for n in scr}
    emit_rwkv_stage1(G, src, srck, gcol, W, rpar_ap, rk_ap, {k: v for k, v in scr.items() if k != "y"}, scrk, S)
    emit_rwkv_stage2(G, rk_ap, scr, scrk, S)
    emit_rwkv_stage3(G, src, srck, dst, dstk, W["wo"], rpar_ap, rk_ap, scr, scrk, S)


def test_rwkv(nc, G, P, Rf, ins, xA, kA, xB, kB, S):
    W = {}
    def din(name, arr):
        arr = np.ascontiguousarray(arr, dtype=np.float32)
        ins[name] = arr
        return nc.dram_tensor(name, list(arr.shape), F32, kind="ExternalInput").ap()
    W["wr"] = din("rw_r", P["rwkv_w_rkv"][0][0])
    W["wk"] = din("rw_k", P["rwkv_w_rkv"][0][1])
    W["wv"] = din("rw_v", P["rwkv_w_rkv"][0][2])
    W["wla"] = din("rw_wla", P["rwkv_w_lora_a"][0])
    W["wlb"] = din("rw_wlb", P["rwkv_w_lora_b"][0])
    W["ala"] = din("rw_ala", P["rwkv_a_lora_a"][0])
    W["alb"] = din("rw_alb", P["rwkv_a_lora_b"][0])
    W["gla"] = din("rw_gla", P["rwkv_g_lora_a"][0])
    W["glb"] = din("rw_glb", P["rwkv_g_lora_b"][0])
    W["wo"] = din("rw_wo", P["rwkv_w_o"][0])
    rpar = din("rw_par", make_rwkv_params(P["rwkv_mix"][0], P["rwkv_w0"][0], P["rwkv_a0"][0], P["rwkv_k_k"][0], P["rwkv_k_a"][0],
                                          P["rwkv_r_k"][0], P["rwkv_ln_w"][0], P["rwkv_ln_b"][0]))
    rk = din("rw_cst", make_rwkv_consts())
    emit_rwkv(G, nc, xA, kA, xB, kB, 0, W, rpar, rk, S)


DEPTH = 4
SEQ = 4096
NCORES = 8
LAUNCH_PLAN = [[0, 1, 2, 3]]


def build_module(S=SEQ, depth=DEPTH, layers=None):
    steps = expand_steps(layers if layers is not None else range(depth))
    nc = bass.Bass("TRN2", target_bir_lowering=False)
    dt = lambda name, shape, dtype=F32: nc.dram_tensor(name, list(shape), dtype, kind="ExternalInput").ap()
    x = dt("x", [S, D])
    consts = dt("consts", [128, NCONST])
    params = dt("params", [128, 48 * DEPTH])
    out = nc.dram_tensor("out", [S, D], F32, kind="ExternalOutput").ap()
    bufs = [nc.dram_tensor(n, [D, S], F32, kind="Internal").ap() for n in ("xA", "xB")]
    G = setup_globals(nc, consts, params, 48 * DEPTH)
    nt = S // TT
    ks = [[G.kb.tk(f"x{b}") for _ in range(nt)] for b in range(2)]
    cur = 0
    emit_transpose_in(G, x, bufs[0], ks[0], S)
    for st, l in steps:
        kind, j = l % 3, l // 3
        if st == "f1":
            emit_ffn(G, bufs[cur], ks[cur], bufs[1 - cur], ks[1 - cur], 48 * l, dt(f"ffn1_w_gu_{l}", [D, 2 * FF]), dt(f"ffn1_w_down_{l}", [FF, D]), S)
        elif st == "f2":
            emit_ffn(G, bufs[cur], ks[cur], bufs[1 - cur], ks[1 - cur], 48 * l + 32, dt(f"ffn2_w_gu_{l}", [D, 2 * FF]), dt(f"ffn2_w_down_{l}", [FF, D]), S)
        elif kind == 0:
            emit_mlstm(G, bufs[cur], ks[cur], bufs[1 - cur], ks[1 - cur], 48 * l + 16, dt(f"mlstm_w_in_{j}", [D, M_INCOLS]),
                       dt(f"mlstm_w_out_{j}", [D, D]), dt(f"mpar_{j}", [128, MP_N]), S)
        elif kind == 1:
            emit_attn(G, bufs[cur], ks[cur], bufs[1 - cur], ks[1 - cur], 48 * l + 16, dt("attn_w_qkv", [D, A_QKV]), dt("attn_w_o", [D, D]),
                      dt("apar", [128, AP_N]), dt("posb", [128, S], I32), S)
        else:
            RW = {k: dt("rwkv_" + k, s) for k, s in RW_SHAPES.items()}
            emit_rwkv(G, nc, bufs[cur], ks[cur], bufs[1 - cur], ks[1 - cur], 48 * l + 16, RW, dt("rpar", [128, RP_N]), dt("rcst", [128, RK_N]), S)
        cur = 1 - cur
    emit_transpose_out(G, bufs[cur], ks[cur], out, S)
    G.kb.barrier()
    return nc


def expand_steps(layers):
    steps = []
    for it in layers:
        if isinstance(it, (tuple, list)):
            steps.append((it[0], it[1]))
        else:
            steps += [("f1", it), ("mx", it), ("f2", it)]
    return steps


RW_SHAPES = {"wr": [D, D], "wk": [D, D], "wv": [D, D], "wla": [D, 96], "wlb": [96, D], "ala": [D, 96], "alb": [96, D],
             "gla": [D, 256], "glb": [256, D], "wo": [D, D]}


def step_inputs(inp, st, l):
    f32 = lambda a: np.ascontiguousarray(np.asarray(a), dtype=np.float32)
    m = {}
    kind, j = l % 3, l // 3
    if st in ("f1", "f2"):
        pre = "ffn1" if st == "f1" else "ffn2"
        m[f"{pre}_w_gu_{l}"] = f32(inp[f"{pre}_w_gu"][l])
        m[f"{pre}_w_down_{l}"] = f32(inp[f"{pre}_w_down"][l])
    elif kind == 0:
        m[f"mlstm_w_in_{j}"] = f32(inp["mlstm_w_in"][j])
        m[f"mlstm_w_out_{j}"] = f32(inp["mlstm_w_out"][j])
        m[f"mpar_{j}"] = make_mlstm_params(np.asarray(inp["mlstm_b_gate"][j]), np.asarray(inp["mlstm_head_gain"][j]))
    elif kind == 1:
        m["attn_w_qkv"] = f32(inp["attn_w_qkv"][j])
        m["attn_w_o"] = f32(inp["attn_w_o"][j])
        m["apar"] = make_attn_params(np.asarray(inp["attn_q_gain"][j]), np.asarray(inp["attn_k_gain"][j]), np.asarray(inp["attn_sinks"][j]))
    else:
        rkv = np.asarray(inp["rwkv_w_rkv"][j])
        m["rwkv_wr"], m["rwkv_wk"], m["rwkv_wv"] = f32(rkv[0]), f32(rkv[1]), f32(rkv[2])
        for k, n in (("wla", "rwkv_w_lora_a"), ("wlb", "rwkv_w_lora_b"), ("ala", "rwkv_a_lora_a"), ("alb", "rwkv_a_lora_b"),
                     ("gla", "rwkv_g_lora_a"), ("glb", "rwkv_g_lora_b"), ("wo", "rwkv_w_o")):
            m["rwkv_" + k] = f32(inp[n][j])
        m["rpar"] = make_rwkv_params(*[np.asarray(inp[k][j]) for k in ("rwkv_mix", "rwkv_w0", "rwkv_a0", "rwkv_k_k", "rwkv_k_a", "rwkv_r_k",
                                                                      "rwkv_ln_w", "rwkv_ln_b")])
        m["rcst"] = make_rwkv_consts()
    return m


def kernel(**inp):
    f32 = lambda a: np.ascontiguousarray(np.asarray(a), dtype=np.float32)
    lay = lambda v: np.ascontiguousarray(np.asarray(v, np.float32).reshape(16, 128).T)
    params = np.zeros((128, 48 * DEPTH), np.float32)
    for l in range(DEPTH):
        params[:, 48 * l:48 * l + 16] = lay(inp["ffn1_norm"][l])
        params[:, 48 * l + 16:48 * l + 32] = lay(inp["mixer_norm"][l])
        params[:, 48 * l + 32:48 * l + 48] = lay(inp["ffn2_norm"][l])
    x = np.asarray(inp["x"])
    pos = np.asarray(inp["positions"]).astype(np.int32)
    outs = [f32(x[b]) for b in range(NCORES)]
    for layers in LAUNCH_PLAN:
        shared = {"consts": make_consts(), "params": params}
        steps = expand_steps(layers)
        for st, l in steps:
            shared.update(step_inputs(inp, st, l))
        in_maps = []
        for b in range(NCORES):
            m = dict(shared)
            m["x"] = outs[b]
            if any(st == "mx" and l % 3 == 1 for st, l in steps):
                m["posb"] = np.ascontiguousarray(np.broadcast_to(pos[b][None, :], (128, SEQ)))
            in_maps.append(m)
        nc = build_module(layers=layers)
        res = run_bass_kernel_spmd(nc, in_maps, core_ids=list(range(NCORES)))
        outs = [np.ascontiguousarray(np.asarray(r["out"]), dtype=np.float32) for r in res.results]
    return np.stack(outs).astype(np.float32)
```
